# Optimizing a Trainium2 kernel written in Bass

```python
import math
import jax, jax.numpy as jnp
from jax import lax
import numpy as np

D_MODEL = 1024
BATCH = 4
SEQ = 8192
DEPTH = 1

D_MIX = D_MODEL
D_POOL = D_MIX // 2
POOL_WINDOWS = (2, 4, 8, 16)
N_POOL_GROUPS = len(POOL_WINDOWS)
POOL_GROUP = D_POOL // N_POOL_GROUPS
D_ATTN = D_MIX - D_POOL
HEAD_DIM = 64
N_ATTN_HEADS = D_ATTN // (2 * HEAD_DIM)
D_IN = D_POOL + 3 * D_ATTN
ROPE_THETA = 10000.0
Q_BLOCK = 128
LAMBDA_STD = 0.1
D_PLE = 256
N_KEYS = 128
N_EXPERTS = N_KEYS * N_KEYS
N_PEER_HEADS = 8
PEER_TOPK = 16
D_PEER_QUERY = 256
D_SUBKEY = D_PEER_QUERY // 2
PEER_CHUNK = 128
EPS = 1e-6

kernel_name = "hybrid_pool_diffattn_peer_block"


def rmsnorm(x, gain):
    xf = x.astype(jnp.float32)
    y = xf * lax.rsqrt(jnp.mean(xf * xf, axis=-1, keepdims=True) + EPS)
    return (y * gain.astype(jnp.float32)).astype(x.dtype)


def rope(t, positions):
    d = t.shape[-1]
    inv_freq = ROPE_THETA ** (-jnp.arange(0, d, 2, dtype=jnp.float32) / d)
    ang = positions.astype(jnp.float32)[..., None] * inv_freq
    cos = jnp.cos(ang)[:, :, None, None, :]
    sin = jnp.sin(ang)[:, :, None, None, :]
    tf = t.astype(jnp.float32)
    t1, t2 = tf[..., : d // 2], tf[..., d // 2:]
    return jnp.concatenate([t1 * cos - t2 * sin, t2 * cos + t1 * sin], axis=-1).astype(t.dtype)


def pool_mixer(z, pool_w, pool_scale):
    B, S, _ = z.shape
    zg = z.astype(jnp.float32).reshape(B, S, N_POOL_GROUPS, POOL_GROUP)
    csum = lax.cumsum(zg, axis=1)
    t = jnp.arange(1, S + 1, dtype=jnp.float32)
    outs = []
    for g, w in enumerate(POOL_WINDOWS):
        cg = csum[:, :, g]
        prev = jnp.pad(cg, ((0, 0), (w, 0), (0, 0)))[:, :S]
        cnt = jnp.minimum(t, float(w))[None, :, None]
        outs.append((cg - prev) / cnt - zg[:, :, g])
    m = jnp.stack(outs, axis=2).astype(z.dtype)
    y = jnp.einsum('bsgc,gce->bsge', m, pool_w).reshape(B, S, D_POOL)
    return y * pool_scale


def diff_attention(q, k, v, positions, q_norm, k_norm, lq1, lk1, lq2, lk2, subln, layer_idx):
    B, S, H, _, d = q.shape
    q = rope(rmsnorm(q, q_norm), positions)
    k = rope(rmsnorm(k, k_norm), positions)
    lam_init = 0.8 - 0.6 * math.exp(-0.3 * layer_idx)
    lam = (jnp.exp(jnp.sum(lq1.astype(jnp.float32) * lk1.astype(jnp.float32)))
           - jnp.exp(jnp.sum(lq2.astype(jnp.float32) * lk2.astype(jnp.float32))) + lam_init)
    scale = 1.0 / math.sqrt(d)
    nb = S // Q_BLOCK
    qb = q.reshape(B, nb, Q_BLOCK, H, 2, d).swapaxes(0, 1)
    kf = k.astype(jnp.float32)
    vf = v.astype(jnp.float32)
    key_pos = jnp.arange(S)

    def block(args):
        q_blk, bi = args
        s = jnp.einsum('bqhmd,bkhmd->bhmqk', q_blk.astype(jnp.float32), kf) * scale
        qpos = bi * Q_BLOCK + jnp.arange(Q_BLOCK)
        mask = key_pos[None, :] <= qpos[:, None]
        s = jnp.where(mask, s, -jnp.inf)
        pr = jax.nn.softmax(s, axis=-1)
        a = pr[:, :, 0] - lam * pr[:, :, 1]
        return jnp.einsum('bhqk,bkhe->bqhe', a, vf)

    o = lax.map(block, (qb, jnp.arange(nb)))
    o = o.swapaxes(0, 1).reshape(B, S, H, 2 * d)
    o = rmsnorm(o, subln) * (1.0 - lam_init)
    return o.reshape(B, S, H * 2 * d).astype(v.dtype)


def peer(xn, w_q, subkeys, expert_u, expert_v):
    B, S, D = xn.shape
    q = (xn @ w_q).reshape(B, S, N_PEER_HEADS, 2, D_SUBKEY)
    sc = jnp.einsum('bshmc,hmnc->bshmn', q.astype(jnp.float32), subkeys.astype(jnp.float32))
    v_half, i_half = lax.top_k(sc, PEER_TOPK)
    cand = v_half[..., 0, :, None] + v_half[..., 1, None, :]
    cidx = i_half[..., 0, :, None] * N_KEYS + i_half[..., 1, None, :]
    cand = cand.reshape(B, S, N_PEER_HEADS, PEER_TOPK * PEER_TOPK)
    cidx = cidx.reshape(B, S, N_PEER_HEADS, PEER_TOPK * PEER_TOPK)
    top_s, pos = lax.top_k(cand, PEER_TOPK)
    eidx = jnp.take_along_axis(cidx, pos, axis=-1)
    g = jax.nn.softmax(top_s, axis=-1)
    nc = (B * S) // PEER_CHUNK
    xc = xn.reshape(nc, PEER_CHUNK, D)
    ic = eidx.reshape(nc, PEER_CHUNK, N_PEER_HEADS * PEER_TOPK)
    gc = g.reshape(nc, PEER_CHUNK, N_PEER_HEADS * PEER_TOPK).astype(xn.dtype)

    def chunk(args):
        xt, it, gt = args
        u = jnp.take(expert_u, it, axis=0)
        a = jnp.einsum('tc,tec->te', xt, u)
        hid = jax.nn.gelu(a, approximate=False) * gt
        vv = jnp.take(expert_v, it, axis=0)
        return jnp.einsum('te,tec->tc', hid, vv)

    out = lax.map(chunk, (xc, ic, gc))
    return out.reshape(B, S, D)


def setup_inputs(seed: int = 0) -> dict:
    key = jax.random.key(seed)
    ks = jax.random.split(key, 24)
    f32 = jnp.float32
    nrm = lambda k, shape, std: jax.random.normal(k, shape, f32) * std
    gain = lambda k, shape: 1.0 + 0.02 * jax.random.normal(k, shape, f32)
    return {
        "x": jax.random.normal(ks[0], (BATCH, SEQ, D_MODEL), f32),
        "p": jax.random.normal(ks[1], (DEPTH, BATCH, SEQ, D_PLE), f32),
        "positions": jnp.broadcast_to(jnp.arange(SEQ, dtype=jnp.int32), (BATCH, SEQ)),
        "ln_mix": gain(ks[2], (DEPTH, D_MODEL)),
        "w_in": nrm(ks[3], (DEPTH, D_MODEL, D_IN), D_MODEL ** -0.5),
        "pool_w": nrm(ks[4], (DEPTH, N_POOL_GROUPS, POOL_GROUP, POOL_GROUP), POOL_GROUP ** -0.5),
        "pool_scale": gain(ks[5], (DEPTH, D_POOL)),
        "q_norm": gain(ks[6], (DEPTH, HEAD_DIM)),
        "k_norm": gain(ks[7], (DEPTH, HEAD_DIM)),
        "lambda_q1": nrm(ks[8], (DEPTH, HEAD_DIM), LAMBDA_STD),
        "lambda_k1": nrm(ks[9], (DEPTH, HEAD_DIM), LAMBDA_STD),
        "lambda_q2": nrm(ks[10], (DEPTH, HEAD_DIM), LAMBDA_STD),
        "lambda_k2": nrm(ks[11], (DEPTH, HEAD_DIM), LAMBDA_STD),
        "subln": gain(ks[12], (DEPTH, 2 * HEAD_DIM)),
        "w_o": nrm(ks[13], (DEPTH, D_MIX, D_MODEL), D_MIX ** -0.5),
        "ln_ffn": gain(ks[14], (DEPTH, D_MODEL)),
        "w_peer_q": nrm(ks[15], (DEPTH, D_MODEL, N_PEER_HEADS * D_PEER_QUERY), D_MODEL ** -0.5),
        "peer_subkeys": nrm(ks[16], (DEPTH, N_PEER_HEADS, 2, N_KEYS, D_SUBKEY), D_SUBKEY ** -0.5),
        "peer_u": nrm(ks[17], (DEPTH, N_EXPERTS, D_MODEL), D_MODEL ** -0.5),
        "peer_v": nrm(ks[18], (DEPTH, N_EXPERTS, D_MODEL), N_PEER_HEADS ** -0.5),
        "ln_pe": gain(ks[19], (DEPTH, D_MODEL)),
        "w_pe_gate": nrm(ks[20], (DEPTH, D_MODEL, D_MODEL), D_MODEL ** -0.5),
        "w_pe_proj": nrm(ks[21], (DEPTH, D_PLE, D_MODEL), D_PLE ** -0.5),
    }


def reference(x, p, positions, ln_mix, w_in, pool_w, pool_scale, q_norm, k_norm,
              lambda_q1, lambda_k1, lambda_q2, lambda_k2, subln, w_o, ln_ffn,
              w_peer_q, peer_subkeys, peer_u, peer_v, ln_pe, w_pe_gate, w_pe_proj):
    B, S, _ = x.shape
    h = x
    for i in range(DEPTH):
        xn = rmsnorm(h, ln_mix[i])
        z = xn @ w_in[i]
        z_pool = z[..., :D_POOL]
        zq = z[..., D_POOL:D_POOL + D_ATTN].reshape(B, S, N_ATTN_HEADS, 2, HEAD_DIM)
        zk = z[..., D_POOL + D_ATTN:D_POOL + 2 * D_ATTN].reshape(B, S, N_ATTN_HEADS, 2, HEAD_DIM)
        zv = z[..., D_POOL + 2 * D_ATTN:].reshape(B, S, N_ATTN_HEADS, 2 * HEAD_DIM)
        y_pool = pool_mixer(z_pool, pool_w[i], pool_scale[i])
        y_attn = diff_attention(zq, zk, zv, positions, q_norm[i], k_norm[i],
                                lambda_q1[i], lambda_k1[i], lambda_q2[i], lambda_k2[i],
                                subln[i], i)
        h = h + jnp.concatenate([y_pool, y_attn], axis=-1) @ w_o[i]
        h = h + peer(rmsnorm(h, ln_ffn[i]), w_peer_q[i], peer_subkeys[i], peer_u[i], peer_v[i])
        gate = jax.nn.sigmoid(rmsnorm(h, ln_pe[i]) @ w_pe_gate[i])
        h = h + gate * (p[i] @ w_pe_proj[i])
    return h
```

```python
import math
import contextlib
import numpy as np
import ml_dtypes
import concourse.bass as bass
import concourse.mybir as mybir
from concourse.bass_utils import run_bass_kernel_spmd

F32 = mybir.dt.float32
BF16 = mybir.dt.bfloat16
I32 = mybir.dt.int32
ALU = mybir.AluOpType
AF = mybir.ActivationFunctionType
AX = mybir.AxisListType
COMPUTE = ("pe", "act", "dve", "pool")
EPS = 1e-6
PI = math.pi


class Buf:
    __slots__ = ("name", "last_w", "readers", "dsem", "dcount")

    def __init__(self, name=""):
        self.name = name
        self.last_w = None
        self.readers = []
        self.dsem = None
        self.dcount = 0


class Instr:
    __slots__ = ("eng", "fn", "deps", "is_dma", "dbuf", "dtarget", "signal", "sigval")

    def __init__(self, eng, fn, is_dma=False):
        self.eng = eng
        self.fn = fn
        self.deps = []
        self.is_dma = is_dma
        self.dbuf = None
        self.dtarget = 0
        self.signal = False
        self.sigval = 0


class Prog:
    def __init__(self, nc):
        self.nc = nc
        self.streams = {e: [] for e in ("pe", "act", "dve", "pool", "sp")}
        self.dma_bufs = []

    def _dep_on(self, ins, prev, kind):
        if prev is None or prev is ins:
            return
        if prev.is_dma:
            ins.deps.append(("d", prev.dbuf, prev.dtarget))
            return
        if prev.eng == ins.eng and not ins.is_dma:
            if ins.eng == "pe" or kind == "war":
                return
        ins.deps.append(("c", prev))

    def _track(self, ins, reads, writes):
        for b in reads:
            self._dep_on(ins, b.last_w, "raw")
        for b in writes:
            self._dep_on(ins, b.last_w, "waw")
            for r in b.readers:
                self._dep_on(ins, r, "war")
        for b in writes:
            b.last_w = ins
            b.readers = []
        for b in reads:
            if b.last_w is not ins:
                b.readers.append(ins)

    def op(self, eng, fn, R=(), W=()):
        ins = Instr(eng, fn)
        self._track(ins, R, W)
        self.streams[eng].append(ins)
        return ins

    def dma(self, out, in_, R=(), W=(), sbuf=None, queue="sp"):
        ins = Instr(queue, lambda e: e.dma_start(out=out, in_=in_), is_dma=True)
        if sbuf.dsem is None:
            sbuf.dsem = len(self.dma_bufs)
            self.dma_bufs.append(sbuf)
        self._track(ins, R, W)
        sbuf.dcount += 16
        ins.dbuf = sbuf
        ins.dtarget = sbuf.dcount
        self.streams[queue].append(ins)
        return ins

    def barrier(self, all_bufs):
        lasts = [self.streams[e][-1] for e in COMPUTE if self.streams[e]]
        for e in ("pe", "act", "dve", "pool", "sp"):
            ins = Instr(e, None)
            for p in lasts:
                if p.eng != e:
                    ins.deps.append(("c", p))
            for b in self.dma_bufs:
                if b.dcount:
                    ins.deps.append(("d", b, b.dcount))
            self.streams[e].append(ins)
        for b in all_bufs:
            b.last_w = None
            b.readers = []

    def mm(self, out, lhsT, rhs, start, stop, R, W, skip=False):
        if skip:
            return self.op("pe", lambda e: e.matmul(out, lhsT, rhs, start=start, stop=stop, skip_group_check=True), R, W)
        return self.op("pe", lambda e: e.matmul(out, lhsT, rhs, start=start, stop=stop), R, W)

    def act(self, out, in_, func, R, W, bias=None, scale=None, accum=None):
        kw = {}
        if bias is not None:
            kw["bias"] = bias
        if scale is not None:
            kw["scale"] = scale
        if accum is not None:
            kw["accum_out"] = accum
        return self.op("act", lambda e: e.activation(out=out, in_=in_, func=func, **kw), R, W)

    def tt(self, eng, out, in0, in1, op, R, W):
        return self.op(eng, lambda e: e.tensor_tensor(out=out, in0=in0, in1=in1, op=op), R, W)

    def ts(self, eng, out, in0, s1, op0, R, W, s2=None, op1=None):
        if op1 is None:
            return self.op(eng, lambda e: e.tensor_scalar(out=out, in0=in0, scalar1=s1, scalar2=None, op0=op0), R, W)
        return self.op(eng, lambda e: e.tensor_scalar(out=out, in0=in0, scalar1=s1, scalar2=s2, op0=op0, op1=op1), R, W)

    def stt(self, eng, out, in0, scalar, in1, op0, op1, R, W):
        return self.op(eng, lambda e: e.scalar_tensor_tensor(out=out, in0=in0, scalar=scalar, in1=in1, op0=op0, op1=op1), R, W)

    def cp(self, eng, out, in_, R, W):
        if eng == "act":
            return self.op("act", lambda e: e.copy(out=out, in_=in_), R, W)
        return self.op(eng, lambda e: e.tensor_copy(out=out, in_=in_), R, W)

    def recip(self, out, in_, R, W):
        return self.op("dve", lambda e: e.reciprocal(out=out, in_=in_), R, W)

    def memset(self, eng, ap, val, W):
        return self.op(eng, lambda e: e.memset(ap, val), (), W)

    def rstd(self, rs, ms, count, R, W):
        n = int(rs.shape[-1])
        self.ts("pool", rs, ms, 1.0 / count, ALU.mult, R, W, s2=EPS, op1=ALU.add)
        self.tt("pool", rs, rs, self.mhalf[:, 0:n], ALU.pow, W, W)

    def emit(self, final_bufs=()):
        nc = self.nc
        for lst in self.streams.values():
            for ins in lst:
                for d in ins.deps:
                    if d[0] == "c":
                        d[1].signal = True
        for e in COMPUTE:
            c = 0
            for ins in self.streams[e]:
                if ins.signal:
                    c += 1
                ins.sigval = c
        with contextlib.ExitStack() as st:
            csem = {e: st.enter_context(nc.semaphore("cs_" + e)) for e in COMPUTE}
            dsem = [st.enter_context(nc.semaphore("ds%d" % i)) for i in range(len(self.dma_bufs))]
            block = st.enter_context(nc.Block())
            engobj = {"pe": "tensor", "act": "scalar", "dve": "vector", "pool": "gpsimd", "sp": "sync"}

            def make(e):
                lst = self.streams[e]

                def body(eng):
                    seen_c = {x: 0 for x in COMPUTE}
                    seen_d = {}
                    for ins in lst:
                        need_c = {}
                        need_d = {}
                        for d in ins.deps:
                            if d[0] == "c":
                                p = d[1]
                                if p.sigval > need_c.get(p.eng, 0):
                                    need_c[p.eng] = p.sigval
                            else:
                                b, t = d[1], d[2]
                                if t > need_d.get(b.dsem, 0):
                                    need_d[b.dsem] = t
                        for pe_, v in need_c.items():
                            if v > seen_c[pe_]:
                                eng.wait_ge(csem[pe_], v)
                                seen_c[pe_] = v
                        for si, v in need_d.items():
                            if v > seen_d.get(si, 0):
                                eng.wait_ge(dsem[si], v)
                                seen_d[si] = v
                        if ins.fn is None:
                            continue
                        bi = ins.fn(eng)
                        if ins.is_dma:
                            bi.then_inc(dsem[ins.dbuf.dsem], 16)
                        elif ins.signal:
                            bi.then_inc(csem[e], 1)
                    if e == "sp":
                        for b in final_bufs:
                            eng.wait_ge(dsem[b.dsem], b.dcount)
                return body

            for e in ("sp", "pe", "act", "dve", "pool"):
                getattr(block, engobj[e])(make(e))


class Arena:
    def __init__(self, ap, words):
        self.ap = ap
        self.words = words
        self.off = 0

    def reset(self, off=0):
        self.hw = max(getattr(self, "hw", 0), self.off)
        self.off = off

    def alloc(self, free_shape, dtype):
        n = 1
        for s in free_shape:
            n *= s
        words = n if dtype in (F32, I32) else (n + 1) // 2
        assert self.off + words <= self.words, ("arena overflow", self.off, words, self.words)
        v = self.ap[:, self.off:self.off + words]
        self.off += words
        if dtype != F32:
            v = v.bitcast(dtype)
            if v.shape[1] != n:
                v = v[:, 0:n]
        if len(free_shape) > 1:
            names = ["a%d" % i for i in range(len(free_shape))]
            v = v.rearrange("p (%s) -> p %s" % (" ".join(names), " ".join(names)),
                            **{names[i]: free_shape[i] for i in range(len(free_shape))})
        return v


class Ring:
    def __init__(self, arena, n, free_shape, dtype, name):
        self.items = [(arena.alloc(free_shape, dtype), Buf("%s%d" % (name, i))) for i in range(n)]
        self.i = 0

    def next(self):
        it = self.items[self.i % len(self.items)]
        self.i += 1
        return it

    def bufs(self):
        return [b for _, b in self.items]


D = 1024
NEXP = 16384
NCH = 128
GRP = 8
NGRP = NCH // GRP
BIG = 1.0e6


def build(S, phases=(0, 1, 2), debug=False):
    NS = S // 1024
    NO = NS * 512
    NT = S // 128
    nc = bass.Bass("TRN2", target_bir_lowering=False)

    def din(name, shape, dt=F32):
        return nc.dram_tensor(name, shape, dt, kind="ExternalInput").ap()

    xs_d = din("xs", [S, D])
    pos_s_d = din("pos_s", [128, NT], I32)
    xo_d = din("xo", [NO, D])
    pos_o_d = din("pos_o", [128, NS * 4], I32)
    xh_d = din("xh", [NS * 16, D])
    amask_d = din("amask", [NS, 8, 128, 512], BF16)
    bgen_d = din("bgen", [128, 4, 2, 128], BF16)
    bfirst_d = din("bfirst", [NS, 128, 4, 128], BF16)
    po_d = din("po", [NO, 256])
    lnmix_d = din("ln_mix_t", [128, 8])
    w_in_d = din("w_in", [D, 2048])
    poolw_d = din("pool_w", [4, 128, 128])
    rowsc_d = din("rowscale_t", [128, 8])
    qkg_d = din("qk_gain", [128, 2, 64])
    lam_d = din("lam_vecs", [128, 4, 64])
    w_o_d = din("w_o", [D, D])
    lnffn_d = din("ln_ffn_t", [128, 8])
    wq_d = din("w_peer_q", [D, 2048])
    sk_d = din("peer_subkeys", [16, 128, 128])
    u_d = din("peer_u", [NEXP, D])
    v_d = din("peer_v", [NEXP, D])
    lnpe_d = din("ln_pe_t", [128, 8])
    wg_d = din("w_pe_gate", [D, D])
    wp_d = din("w_pe_proj", [256, D])
    ident_d = din("ident", [128, 128])
    invf_d = din("invfreq", [128, 32])
    out_d = nc.dram_tensor("out", [NO, D], F32, kind="ExternalOutput").ap()
    dbg_d = nc.dram_tensor("dbg", [NO, D], F32, kind="ExternalOutput").ap() if debug else None

    def dscr(name, shape, dt):
        return nc.dram_tensor(name, shape, dt, kind="Internal").ap()

    h1_d = dscr("h1s", [NO, D], F32)
    ut_d = dscr("ut16", [D, NEXP], BF16)
    v16_d = dscr("v16", [NEXP, D], BF16)
    wq16_d = dscr("wq16", [D, 2048], BF16)
    wg16_d = dscr("wg16", [D, D], BF16)
    wp16_d = dscr("wp16", [256, D], BF16)
    Bh1d, Butd, Bv16d, Bwq16d, Bwg16d, Bwp16d = [Buf(n) for n in ("h1d", "utd", "v16d", "wq16d", "wg16d", "wp16d")]

    AW = 53100
    with contextlib.ExitStack() as st:
        arena_t = st.enter_context(nc.sbuf_tensor("arena", [128, AW], F32))
        psum_t = [st.enter_context(nc.psum_tensor("pb%d" % i, [128, 512], F32)) for i in range(8)]
        PB = [Buf("psum%d" % i) for i in range(8)]
        P = Prog(nc)
        A = Arena(arena_t[:, :], AW)
        allbufs = list(PB)

        dumps = []

        def dump(name, ap, R):
            if not debug:
                return
            shp = [int(x) for x in ap.shape]
            dd = nc.dram_tensor("dump_" + name, shp, ap.dtype, kind="ExternalOutput").ap()
            b = Buf("dump_" + name)
            P.dma(dd, ap, R=R, sbuf=b)
            dumps.append(b)

        def newbuf(name):
            b = Buf(name)
            allbufs.append(b)
            return b

        def ring(n, shape, dt, name):
            r = Ring(A, n, shape, dt, name)
            allbufs.extend(r.bufs())
            return r

        Bc = newbuf("consts")
        ident = A.alloc([128], BF16)
        lnmix = A.alloc([8], F32)
        rowsc = A.alloc([8], F32)
        lnffn = A.alloc([8], F32)
        lnpe = A.alloc([8], F32)
        qkg = A.alloc([2, 64], F32)
        invf = A.alloc([32], F32)
        posf_s = A.alloc([NT], F32)
        posf_o = A.alloc([NS * 4], F32)
        poolw = A.alloc([4, 128], BF16)
        bgen = A.alloc([4, 2, 128], BF16)
        nlam = A.alloc([1], F32)
        mhalf = A.alloc([8], F32)
        P.mhalf = mhalf
        persist_off = A.off
        ident32 = A.alloc([128], F32)
        lamv = A.alloc([4, 64], F32)
        posi_s = A.alloc([NT], I32)
        posi_o = A.alloc([NS * 4], I32)
        poolw32 = A.alloc([4, 128], F32)
        lam_s = A.alloc([8], F32)
        for dst, src in ((ident32, ident_d), (lnmix, lnmix_d), (rowsc, rowsc_d), (lnffn, lnffn_d), (lnpe, lnpe_d),
                         (qkg, qkg_d), (lamv, lam_d), (invf, invf_d), (posi_s, pos_s_d), (posi_o, pos_o_d),
                         (bgen, bgen_d)):
            P.dma(dst, src, W=[Bc], sbuf=Bc)
        P.dma(poolw32, poolw_d.rearrange("g c e -> c g e"), W=[Bc], sbuf=Bc)
        Bk = newbuf("consts2")
        P.cp("dve", ident, ident32, [Bc], [Bk])
        P.memset("dve", mhalf, -0.5, [Bk])
        P.cp("dve", poolw, poolw32, [Bc], [Bk])
        P.cp("dve", posf_s, posi_s, [Bc], [Bk])
        P.cp("dve", posf_o, posi_o, [Bc], [Bk])
        lam_init = 0.8 - 0.6 * math.exp(-0.3 * 0)
        lprod = A.alloc([2, 64], F32)
        P.tt("dve", lprod[:, 0, :], lamv[:, 0, :], lamv[:, 1, :], ALU.mult, [Bc], [Bk])
        P.tt("dve", lprod[:, 1, :], lamv[:, 2, :], lamv[:, 3, :], ALU.mult, [Bc], [Bk])
        P.op("dve", lambda e: e.tensor_reduce(out=lam_s[:, 0:2], in_=lprod, axis=AX.X, op=ALU.add), [Bk], [Bk])
        P.act(lam_s[:, 2:4], lam_s[:, 0:2], AF.Exp, [Bk], [Bk])
        P.tt("dve", lam_s[:, 4:5], lam_s[:, 3:4], lam_s[:, 2:3], ALU.subtract, [Bk], [Bk])
        P.ts("dve", nlam, lam_s[:, 4:5], -lam_init, ALU.add, [Bk], [Bk])
        P.ts("dve", rowsc[:, 4:8], rowsc[:, 4:8], 1.0 - lam_init, ALU.mult, [Bc], [Bk])
        P.barrier(allbufs)
        A.reset(persist_off)
        CONST = []

        w_in16 = A.alloc([8, 2048], BF16)
        w_o16 = A.alloc([8, 1024], BF16)
        Bwin, Bwo = newbuf("w_in16"), newbuf("w_o16")
        mixer_off = A.off

        if 0 in phases:
            A.reset(mixer_off)
            stage8 = ring(2, [2048], F32, "stage8")
            stage4 = ring(4, [1024], F32, "stage4")
            cast4 = ring(3, [1024], BF16, "cast4")
            utg = ring(2, [8, 1024], BF16, "utg")
            w16s = ring(2, [2048], BF16, "w16s")
            engs2 = ("dve", "pool")
            for K in range(8):
                sa, sbuf_ = stage8.next()
                P.dma(sa, w_in_d[K * 128:(K + 1) * 128, :], W=[sbuf_], sbuf=sbuf_)
                P.ts(engs2[K % 2], w_in16[:, K, :], sa, lnmix[:, K:K + 1], ALU.mult, [sbuf_] + CONST, [Bwin])
            for K in range(8):
                sa, sbuf_ = stage4.next()
                P.dma(sa, w_o_d[K * 128:(K + 1) * 128, :], W=[sbuf_], sbuf=sbuf_)
                P.ts(engs2[K % 2], w_o16[:, K, :], sa, rowsc[:, K:K + 1], ALU.mult, [sbuf_] + CONST, [Bwo])
            for K in range(8):
                sa, sbuf_ = stage8.next()
                P.dma(sa, wq_d[K * 128:(K + 1) * 128, :], W=[sbuf_], sbuf=sbuf_)
                wa, wb = w16s.next()
                P.ts(engs2[K % 2], wa, sa, lnffn[:, K:K + 1], ALU.mult, [sbuf_] + CONST, [wb])
                P.dma(wq16_d[K * 128:(K + 1) * 128, :], wa, R=[wb], W=[Bwq16d], sbuf=wb)
            for K in range(8):
                sa, sbuf_ = stage4.next()
                P.dma(sa, wg_d[K * 128:(K + 1) * 128, :], W=[sbuf_], sbuf=sbuf_)
                wa, wb = cast4.next()
                P.ts(engs2[K % 2], wa, sa, lnpe[:, K:K + 1], ALU.mult, [sbuf_] + CONST, [wb])
                P.dma(wg16_d[K * 128:(K + 1) * 128, :], wa, R=[wb], W=[Bwg16d], sbuf=wb)
            for K in range(2):
                sa, sbuf_ = stage4.next()
                P.dma(sa, wp_d[K * 128:(K + 1) * 128, :], W=[sbuf_], sbuf=sbuf_)
                wa, wb = cast4.next()
                P.cp(engs2[K % 2], wa, sa, [sbuf_], [wb])
                P.dma(wp16_d[K * 128:(K + 1) * 128, :], wa, R=[wb], W=[Bwp16d], sbuf=wb)
            cengs = ("act", "pool", "dve")
            for i in range(NCH):
                sa, sbuf_ = stage4.next()
                P.dma(sa, v_d[i * 128:(i + 1) * 128, :], W=[sbuf_], sbuf=sbuf_)
                wa, wb = cast4.next()
                P.cp(cengs[i % 3], wa, sa, [sbuf_], [wb])
                P.dma(v16_d[i * 128:(i + 1) * 128, :], wa, R=[wb], W=[Bv16d], sbuf=wb)
            for g in range(NGRP):
                ua, ub = utg.next()
                for c in range(GRP):
                    i = g * GRP + c
                    sa, sbuf_ = stage4.next()
                    P.dma(sa, u_d[i * 128:(i + 1) * 128, :], W=[sbuf_], sbuf=sbuf_)
                    wa, wb = cast4.next()
                    P.cp(cengs[i % 3], wa, sa, [sbuf_], [wb])
                    for half in range(2):
                        pbi = (i * 2 + half) % 4
                        for kk in range(4):
                            K = half * 4 + kk
                            P.mm(psum_t[pbi][:, kk * 128:(kk + 1) * 128], wa[:, K * 128:(K + 1) * 128], ident,
                                 True, True, [wb] + CONST, [PB[pbi]])
                        P.cp("act" if half == 0 else "dve", ua[:, half * 4:(half + 1) * 4, c * 128:(c + 1) * 128],
                             psum_t[pbi][:, :].rearrange("p (k e) -> p k e", k=4), [PB[pbi]], [ub])
                P.dma(ut_d[:, g * 1024:(g + 1) * 1024].rearrange("(k p) e -> p k e", p=128), ua, R=[ub], W=[Butd], sbuf=ub)
            P.barrier(allbufs)

        if 1 in phases:
            A.reset(mixer_off)
            KT = A.alloc([2, S], BF16)
            Vp = A.alloc([NT, 2, 130], BF16)
            yT01 = A.alloc([2, NO], BF16)
            BKT = [newbuf("KT%d" % i) for i in range(S // 512)]
            BV = [newbuf("V%d" % i) for i in range(S // 512)]
            ByT01 = [newbuf("yT01_%d" % i) for i in range(NS)]
            Bones = newbuf("vones")
            xt = ring(2, [1024], F32, "xt")
            junk = A.alloc([1024], BF16)
            Bjunk = newbuf("junk")
            xn16 = ring(2, [1024], BF16, "xn16")
            st_r = ring(4, [8], F32, "stat")
            st_n = ring(3, [8], F32, "stat_n")
            st_q = ring(3, [8], F32, "stat_q")
            xnT = ring(2, [8, 512], BF16, "xnT")
            qk32 = ring(2, [256], F32, "qk32")
            qkA = ring(2, [256], F32, "qkA")
            qkB = ring(2, [128], F32, "qkB")
            qk16 = ring(2, [2, 128], BF16, "qk16")
            cs_r = ring(1, [2, 4, 32], F32, "cossin")
            cs_tmp = ring(1, [4, 32], F32, "cstmp")
            cs_tmi = A.alloc([4, 32], I32)
            Bcsi = newbuf("cstmi")
            QT = ring(1, [2, 512], BF16, "QT")
            pt_r = ring(4, [512], BF16, "pt")
            msk_r = ring(1, [8, 512], BF16, "msk")
            o_r = ring(2, [128], F32, "o32")
            y16_r = ring(2, [128], BF16, "y16")
            zt = A.alloc([5, 512], BF16)
            Bzt = newbuf("zt")
            mT = ring(2, [512], BF16, "mT")
            yTb = A.alloc([6, 512], BF16)
            ByTb = newbuf("yTb")
            bfirst = A.alloc([4, 128], BF16)
            Bbf = newbuf("bfirst")
            h1t = ring(1, [1024], F32, "h1t")
            haloT = A.alloc([8, 128], BF16)
            BhaloT = newbuf("haloT")
            ZB = (0, 1)
            TPB = 2
            SB_ = (3, 4)
            OB = (5, 6, 7)
            O_SLOT = [(OB[i // 3], (i % 3) * 160) for i in range(8)]

            P.memset("pool", Vp[:, :, :, 128:130], 1.0, [Bones])

            def nt_load(src_d, row0):
                xa, xb = xt.next()
                P.dma(xa, src_d[row0:row0 + 128, :], W=[xb], sbuf=xb)
                return xa, xb

            def nt_compute(ld, xnT_ap, xnT_buf, j):
                xa, xb = ld
                sa, sbf = st_n.next()
                P.act(junk, xa, AF.Square, [xb], [sbf], accum=sa[:, 0:1])
                yield
                P.rstd(sa[:, 1:2], sa[:, 0:1], 1024, [sbf], [sbf])
                na, nb = xn16.next()
                P.ts("dve", na, xa, sa[:, 1:2], ALU.mult, [xb, sbf], [nb])
                yield
                for half in range(2):
                    for kk in range(4):
                        K = half * 4 + kk
                        P.mm(psum_t[TPB][:, kk * 128:(kk + 1) * 128], na[:, K * 128:(K + 1) * 128], ident, True, True,
                             [nb] + CONST, [PB[TPB]])
                    P.cp("act" if half == 0 else "dve", xnT_ap[:, half * 4:(half + 1) * 4, j * 128:(j + 1) * 128],
                         psum_t[TPB][:, :].rearrange("p (k e) -> p k e", k=4), [PB[TPB]], [xnT_buf])
                yield

            def norm_block(src_d, tile0, xnT_ap, xnT_buf):
                lds = [nt_load(src_d, tile0 * 128), nt_load(src_d, (tile0 + 1) * 128)]
                for j in range(4):
                    yield from nt_compute(lds[j], xnT_ap, xnT_buf, j)
                    if j + 2 < 4:
                        lds.append(nt_load(src_d, (tile0 + j + 2) * 128))

            def cos_sin(posf, col0):
                ca, cb = cs_r.next()
                ta, tb = cs_tmp.next()
                for which, shift in ((1, 0.0), (0, PI / 2)):
                    dst = ca[:, which, :, :]
                    P.tt("dve", dst, posf[:, col0:col0 + 4].unsqueeze(2).to_broadcast([128, 4, 32]),
                         invf.unsqueeze(1).to_broadcast([128, 4, 32]), ALU.mult, CONST, [cb])
                    if shift:
                        P.ts("dve", dst, dst, shift, ALU.add, [cb], [cb])
                    P.ts("dve", ta, dst, 1.0 / (2 * PI), ALU.mult, [cb], [tb])
                    P.cp("dve", cs_tmi, ta, [tb], [Bcsi])
                    P.cp("dve", ta, cs_tmi, [Bcsi], [tb])
                    P.stt("dve", dst, ta, -2 * PI, dst, ALU.mult, ALU.add, [tb, cb], [cb])
                    P.ts("dve", ta, dst, PI, ALU.is_gt, [cb], [tb], s2=-2 * PI, op1=ALU.mult)
                    P.tt("dve", dst, dst, ta, ALU.add, [cb, tb], [cb])
                    P.ts("dve", ta, dst, -PI, ALU.is_lt, [cb], [tb], s2=2 * PI, op1=ALU.mult)
                    P.tt("dve", dst, dst, ta, ALU.add, [cb, tb], [cb])
                P.act(ca, ca, AF.Sin, [cb], [cb])
                return ca, cb

            def qk_norm_rope(zps, zbuf, gi, ca, cb, j):
                sa, sbf = st_r.next()
                q32, q32b = qk32.next()
                P.act(q32, zps, AF.Square, [zbuf], [q32b])
                P.op("dve", lambda e: e.tensor_reduce(out=sa[:, 0:4], in_=q32.rearrange("p (a d) -> p a d", a=4),
                                                      axis=AX.X, op=ALU.add), [q32b], [sbf])
                P.rstd(sa[:, 4:8], sa[:, 0:4], 64, [sbf], [sbf])
                P.tt("dve", q32.rearrange("p (a d) -> p a d", a=4), zps.rearrange("p (a d) -> p a d", a=4),
                     sa[:, 4:8].unsqueeze(2).to_broadcast([128, 4, 64]), ALU.mult, [zbuf, sbf], [q32b])
                P.tt("pool", q32.rearrange("p (a d) -> p a d", a=4), q32.rearrange("p (a d) -> p a d", a=4),
                     qkg[:, gi, :].unsqueeze(1).to_broadcast([128, 4, 64]), ALU.mult, [q32b] + CONST, [q32b])
                qa, qab = qkA.next()
                qb_, qbb = qkB.next()
                v4 = q32.rearrange("p (a h f) -> p a h f", a=4, h=2)
                cosb = ca[:, 0, j, :].unsqueeze(1).to_broadcast([128, 4, 32])
                sinb = ca[:, 1, j, :].unsqueeze(1).to_broadcast([128, 4, 32])
                a4 = qa.rearrange("p (a h f) -> p a h f", a=4, h=2)
                P.tt("dve", a4[:, :, 0, :], v4[:, :, 0, :], cosb, ALU.mult, [q32b, cb], [qab])
                P.tt("pool", a4[:, :, 1, :], v4[:, :, 1, :], cosb, ALU.mult, [q32b, cb], [qab])
                b3 = qb_.rearrange("p (a f) -> p a f", a=4)
                o16, o16b = qk16.next()
                o4 = o16.rearrange("p h (m t f) -> p (h m) t f", m=2, t=2)
                P.tt("pool", b3, v4[:, :, 1, :], sinb, ALU.mult, [q32b, cb], [qbb])
                P.tt("dve", o4[:, :, 0, :], a4[:, :, 0, :], b3, ALU.subtract, [qab, qbb], [o16b])
                P.tt("pool", b3, v4[:, :, 0, :], sinb, ALU.mult, [q32b, cb, o16b], [qbb])
                P.tt("dve", o4[:, :, 1, :], a4[:, :, 1, :], b3, ALU.add, [qab, qbb], [o16b])
                return o16, o16b

            def qk_norm_rope_gen(zps, zbuf, gi, ca, cb, j, res):
                sa, sbf = st_q.next()
                q32, q32b = qk32.next()
                q3 = q32.rearrange("p (a d) -> p a d", a=4)
                P.act(q32, zps, AF.Square, [zbuf], [q32b])
                yield
                P.op("dve", lambda e: e.tensor_reduce(out=sa[:, 0:4], in_=q3, axis=AX.X, op=ALU.add), [q32b], [sbf])
                P.rstd(sa[:, 4:8], sa[:, 0:4], 64, [sbf], [sbf])
                yield
                P.tt("dve", q3, zps.rearrange("p (a d) -> p a d", a=4),
                     sa[:, 4:8].unsqueeze(2).to_broadcast([128, 4, 64]), ALU.mult, [zbuf, sbf], [q32b])
                yield
                P.tt("pool", q3, q3, qkg[:, gi, :].unsqueeze(1).to_broadcast([128, 4, 64]), ALU.mult, [q32b] + CONST, [q32b])
                yield
                qa, qab = qkA.next()
                qb_, qbb = qkB.next()
                v4 = q32.rearrange("p (a h f) -> p a h f", a=4, h=2)
                cosb = ca[:, 0, j, :].unsqueeze(1).to_broadcast([128, 4, 32])
                sinb = ca[:, 1, j, :].unsqueeze(1).to_broadcast([128, 4, 32])
                a4 = qa.rearrange("p (a h f) -> p a h f", a=4, h=2)
                P.tt("dve", a4[:, :, 0, :], v4[:, :, 0, :], cosb, ALU.mult, [q32b, cb], [qab])
                P.tt("pool", a4[:, :, 1, :], v4[:, :, 1, :], cosb, ALU.mult, [q32b, cb], [qab])
                b3 = qb_.rearrange("p (a f) -> p a f", a=4)
                o16, o16b = qk16.next()
                o4 = o16.rearrange("p h (m t f) -> p (h m) t f", m=2, t=2)
                P.tt("pool", b3, v4[:, :, 1, :], sinb, ALU.mult, [q32b, cb], [qbb])
                yield
                P.tt("dve", o4[:, :, 0, :], a4[:, :, 0, :], b3, ALU.subtract, [qab, qbb], [o16b])
                P.tt("pool", b3, v4[:, :, 0, :], sinb, ALU.mult, [q32b, cb, o16b], [qbb])
                yield
                P.tt("dve", o4[:, :, 1, :], a4[:, :, 1, :], b3, ALU.add, [qab, qbb], [o16b])
                res.append((o16, o16b))

            def chain(*gens):
                for g_ in gens:
                    yield from g_

            def lockstep(gens):
                gens = list(gens)
                while gens:
                    nxt = []
                    for gz in gens:
                        try:
                            next(gz)
                            nxt.append(gz)
                        except StopIteration:
                            pass
                    gens = nxt
                    yield

            for sw in range(2):
                h0 = 2 * sw
                kcol = 1024 + h0 * 128
                vcol = 1536 + h0 * 128
                qcol = 512 + h0 * 128

                def qkv_tile(kind, src_d, tile_idx, j, xa_, xb_, ca, cb, dst, dstbuf, vblk=None):
                    ld = nt_load(src_d, tile_idx * 128)
                    yield
                    yield from nt_compute(ld, xa_, xb_, j)
                    zb = ZB[j % 2]
                    col = kcol if kind == "k" else qcol
                    for K in range(8):
                        P.mm(psum_t[zb][:, 0:256], xa_[:, K, j * 128:(j + 1) * 128], w_in16[:, K, col:col + 256],
                             K == 0, K == 7, [xb_, Bwin], [PB[zb]])
                    if kind == "k":
                        for K in range(8):
                            P.mm(psum_t[zb][:, 256:512], xa_[:, K, j * 128:(j + 1) * 128], w_in16[:, K, vcol:vcol + 256],
                                 K == 0, K == 7, [xb_, Bwin], [PB[zb]])
                    yield
                    if kind == "k":
                        P.cp("act", Vp[:, tile_idx, :, 0:128], psum_t[zb][:, 256:512].rearrange("p (h e) -> p h e", h=2),
                             [PB[zb]], [BV[vblk]])
                    res = []
                    yield from qk_norm_rope_gen(psum_t[zb][:, 0:256], PB[zb], 1 if kind == "k" else 0, ca, cb, j, res)
                    o16, o16b = res[0]
                    yield
                    for hh in range(2):
                        P.mm(psum_t[TPB][:, hh * 128:(hh + 1) * 128], o16[:, hh, :], ident, True, True,
                             [o16b] + CONST, [PB[TPB]])
                    c0 = tile_idx * 128 if kind == "k" else j * 128
                    P.cp("act", dst[:, :, c0:c0 + 128],
                         psum_t[TPB][:, 0:256].rearrange("p (h e) -> p h e", h=2), [PB[TPB]], [dstbuf])
                    yield

                def kv_block(blk):
                    xa_, xb_ = xnT.next()
                    ca, cb = cos_sin(posf_s, blk * 4)
                    yield
                    for pair in ((0, 1), (2, 3)):
                        yield from lockstep([qkv_tile("k", xs_d, blk * 4 + j, j, xa_, xb_, ca, cb, KT, BKT[blk], vblk=blk)
                                             for j in pair])

                def pool_gen(s, xa_, xb_):
                    xh_t, Bxh = h1t.next()
                    P.memset("pool", xh_t, 0.0, [Bxh])
                    P.dma(xh_t[112:128, :], xh_d[s * 16:(s + 1) * 16, :], W=[Bxh], sbuf=Bxh)
                    P.dma(bfirst, bfirst_d[s], W=[Bbf], sbuf=Bbf)
                    ha_, hb_ = haloT, BhaloT
                    yield
                    sa, sbf = st_n.next()
                    P.act(junk, xh_t, AF.Square, [Bxh], [sbf], accum=sa[:, 0:1])
                    yield
                    P.rstd(sa[:, 1:2], sa[:, 0:1], 1024, [sbf], [sbf])
                    na, nb = xn16.next()
                    P.ts("dve", na, xh_t, sa[:, 1:2], ALU.mult, [Bxh, sbf], [nb])
                    yield
                    for half in range(2):
                        for kk in range(4):
                            K = half * 4 + kk
                            P.mm(psum_t[TPB][:, kk * 128:(kk + 1) * 128], na[:, K * 128:(K + 1) * 128], ident,
                                 True, True, [nb] + CONST, [PB[TPB]])
                        P.cp("act" if half == 0 else "dve", ha_[:, half * 4:(half + 1) * 4, 0:128],
                             psum_t[TPB][:, :].rearrange("p (k e) -> p k e", k=4), [PB[TPB]], [hb_])
                    yield
                    for jj in range(5):
                        zb = ZB[jj % 2]
                        src, srcb, col = (ha_, hb_, 0) if jj == 0 else (xa_, xb_, (jj - 1) * 128)
                        for K in range(8):
                            P.mm(psum_t[zb][:, :], src[:, K, col:col + 128], w_in16[:, K, 0:512], K == 0, K == 7,
                                 [srcb, Bwin], [PB[zb]])
                        yield
                        P.cp("act" if jj % 2 else "dve", zt[:, jj, :], psum_t[zb][:, :], [PB[zb]], [Bzt])
                        yield
                    for g in range(4):
                        zb = ZB[g % 2]
                        for j in range(4):
                            cur = bfirst[:, g, :] if j == 0 else bgen[:, g, 1, :]
                            P.mm(psum_t[zb][:, j * 128:(j + 1) * 128], zt[:, j, g * 128:(g + 1) * 128], bgen[:, g, 0, :],
                                 True, False, [Bzt] + CONST, [PB[zb]])
                            P.mm(psum_t[zb][:, j * 128:(j + 1) * 128], zt[:, j + 1, g * 128:(g + 1) * 128], cur,
                                 False, True, [Bzt, Bbf] + CONST, [PB[zb]])
                        yield
                        ma_, mb_ = mT.next()
                        P.cp("act", ma_, psum_t[zb][:, :], [PB[zb]], [mb_])
                        yield
                        P.mm(psum_t[TPB][:, :], poolw[:, g, :], ma_, True, True, [mb_] + CONST, [PB[TPB]])
                        P.cp("dve", yTb[:, g, :], psum_t[TPB][:, :], [PB[TPB]], [ByTb])
                        yield

                def own_slot(s, side=None):
                    xa_, xb_ = xnT.next()
                    ca, cb = cos_sin(posf_o, s * 4)
                    qT, qTb = QT.next()
                    ma, mb = msk_r.next()
                    P.dma(ma, amask_d[s].rearrange("k p q -> p k q"), W=[mb], sbuf=mb)
                    for pair in ((0, 1), (2, 3)):
                        for _ in lockstep([qkv_tile("q", xo_d, s * 4 + j, j, xa_, xb_, ca, cb, qT, qTb) for j in pair]):
                            pass
                    if sw == 1:
                        side = chain(pool_gen(s, xa_, xb_), side) if side is not None else pool_gen(s, xa_, xb_)
                    nkb = 2 * s + 2
                    n_side = 3 if s < 2 else (2 if s < 4 else 1)
                    for hh in range(2):
                        nkt = nkb * 4

                        def emit_pv(g, lst):
                            kb = g // 4
                            for m, pa, pb_ in lst:
                                for qt in range(4):
                                    ob, oc = O_SLOT[qt * 2 + m]
                                    P.mm(psum_t[ob][:, oc:oc + 130], pa[:, qt * 128:(qt + 1) * 128], Vp[:, g, hh, :],
                                         g == 0 and (qt * 2 + m) in (0, 4, 6), g == nkt - 1, [pb_, BV[kb], Bones], [PB[ob]],
                                         skip=True)

                        pend = None
                        for g in range(nkt):
                            kb = g // 4
                            cur = []
                            for m in range(2):
                                sb_i = SB_[m]
                                P.mm(psum_t[sb_i][:, :], KT[m * 64:(m + 1) * 64, hh, g * 128:(g + 1) * 128],
                                     qT[m * 64:(m + 1) * 64, hh, :], True, True, [BKT[kb], qTb], [PB[sb_i]])
                                pa, pb_ = pt_r.next()
                                P.act(pa, psum_t[sb_i][:, :], AF.Exp, [PB[sb_i]], [pb_], scale=0.125)
                                if kb >= 2 * s:
                                    P.tt("dve" if m == 0 else "pool", pa, pa, ma[:, g - 8 * s, :], ALU.mult, [pb_, mb], [pb_])
                                cur.append((m, pa, pb_))
                            if pend is not None:
                                emit_pv(*pend)
                            pend = (g, cur)
                            if side is not None:
                                for _ in range(n_side):
                                    next(side, None)
                        emit_pv(*pend)
                        def fin_tile(qt, hh=hh):
                            ob1, oc1 = O_SLOT[qt * 2]
                            ob2, oc2 = O_SLOT[qt * 2 + 1]
                            sa, sbf = st_r.next()
                            P.recip(sa[:, 0:1], psum_t[ob1][:, oc1 + 128:oc1 + 129], [PB[ob1]], [sbf])
                            P.recip(sa[:, 1:2], psum_t[ob2][:, oc2 + 128:oc2 + 129], [PB[ob2]], [sbf])
                            yield
                            P.tt("dve", sa[:, 2:3], sa[:, 1:2], nlam, ALU.mult, [sbf] + CONST, [sbf])
                            oa, oab = o_r.next()
                            P.ts("dve", oa, psum_t[ob1][:, oc1:oc1 + 128], sa[:, 0:1], ALU.mult, [PB[ob1], sbf], [oab])
                            yield
                            P.stt("dve", oa, psum_t[ob2][:, oc2:oc2 + 128], sa[:, 2:3], oa, ALU.mult, ALU.add,
                                  [PB[ob2], sbf, oab], [oab])
                            yield
                            P.act(junk[:, 0:128], oa, AF.Square, [oab], [sbf], accum=sa[:, 3:4])
                            yield
                            P.rstd(sa[:, 4:5], sa[:, 3:4], 128, [sbf], [sbf])
                            yield
                            ya, yb = y16_r.next()
                            P.ts("dve", ya, oa, sa[:, 4:5], ALU.mult, [oab, sbf], [yb])
                            yield
                            P.mm(psum_t[TPB][:, 256:384], ya, ident, True, True, [yb] + CONST, [PB[TPB]])
                            if sw == 0:
                                P.cp("act", yT01[:, hh, s * 512 + qt * 128:s * 512 + (qt + 1) * 128],
                                     psum_t[TPB][:, 256:384], [PB[TPB]], [ByT01[s]])
                            else:
                                P.cp("act", yTb[:, 4 + hh, qt * 128:(qt + 1) * 128], psum_t[TPB][:, 256:384],
                                     [PB[TPB]], [ByTb])
                            yield

                        for pair in ((0, 1), (2, 3)):
                            for _ in lockstep([fin_tile(qt) for qt in pair]):
                                pass
                    if side is not None:
                        for _ in side:
                            pass
                    if sw == 1:
                        for qt in range(4):
                            ha, hb = h1t.next()
                            P.dma(ha, xo_d[(s * 4 + qt) * 128:(s * 4 + qt + 1) * 128, :], W=[hb], sbuf=hb)
                            for half in range(2):
                                zb = ZB[half]
                                for c in range(8):
                                    if c in (4, 5):
                                        lhs = yT01[:, c - 4, s * 512 + qt * 128:s * 512 + (qt + 1) * 128]
                                        rb = ByT01[s]
                                    else:
                                        lhs = yTb[:, c if c < 4 else c - 2, qt * 128:(qt + 1) * 128]
                                        rb = ByTb
                                    P.mm(psum_t[zb][:, :], lhs, w_o16[:, c, half * 512:(half + 1) * 512], c == 0, c == 7,
                                         [rb, Bwo], [PB[zb]])
                            for half in range(2):
                                P.tt("dve", ha[:, half * 512:(half + 1) * 512], psum_t[ZB[half]][:, :],
                                     ha[:, half * 512:(half + 1) * 512], ALU.add, [PB[ZB[half]], hb], [hb])
                            row = (s * 4 + qt) * 128
                            P.dma(h1_d[row:row + 128, :], ha, R=[hb], W=[Bh1d], sbuf=hb)

                for _ in chain(kv_block(0), kv_block(1)):
                    pass
                for s in range(NS):
                    side = chain(kv_block(2 * s + 2), kv_block(2 * s + 3)) if s + 1 < NS else None
                    own_slot(s, side)
                if sw == 0:
                    dump("KT", KT, BKT)
                    dump("Vp", Vp, BV + [Bones])
                    dump("yT01", yT01, ByT01)
            P.barrier(allbufs)

        final_bufs = []
        if 2 in phases:
            A.reset(persist_off)
            acc = A.alloc([4, 1024], F32)
            Bacc = [newbuf("acc%d" % j) for j in range(4)]
            xnT2 = A.alloc([8, 512], BF16)
            BxnT2 = newbuf("xnT2")
            s2 = A.alloc([4, 8, 128], F32)
            y1 = A.alloc([4, 8, 128], F32)
            Bs2 = [newbuf("s2_%d" % j) for j in range(4)]
            By1 = [newbuf("y1_%d" % j) for j in range(4)]
            kap = A.alloc([4, 8], F32)
            Bkap = [newbuf("kap%d" % j) for j in range(4)]
            skT = A.alloc([16, 128], BF16)
            BskT = newbuf("skT")
            sk32 = ring(2, [128], F32, "sk32")
            sk16 = ring(2, [128], BF16, "sk16")
            off0 = A.off
            ut_r = ring(2, [8, 1024], BF16, "ut")
            off1 = A.off
            v_r = ring(2, [8, 1024], BF16, "vg")
            Wq = arena_t[:, off0:off1].bitcast(BF16).rearrange("p (k c) -> p k c", k=8)
            Wg = arena_t[:, off1:A.off].bitcast(BF16).rearrange("p (k c) -> p k c", k=8)
            BWq = ut_r.bufs()
            BWg = v_r.bufs()
            ga_r = ring(2, [8, 512], BF16, "ga")
            hid_r = ring(1, [8, 512], BF16, "hid")
            Bhid = [newbuf("hid_t%d" % j) for j in range(4)]
            D_r = ring(4, [8, 128], BF16, "Dw")
            E_r = ring(4, [8, 128], BF16, "Ew")
            qTc = ring(2, [512], BF16, "qTc")
            junk2 = A.alloc([1024], BF16)
            Bjunk2 = newbuf("junk2")
            xn16b = ring(2, [1024], BF16, "xn16b")
            st2 = ring(4, [8], F32, "st2")
            tk_v = A.alloc([16, 16], F32)
            tk_tmp = ring(2, [128], F32, "tktmp")
            Btkv = newbuf("tkv")
            cand = A.alloc([8, 256], F32)
            Bcand = newbuf("cand")
            ctmp = ring(2, [256], F32, "ctmp")
            csort = A.alloc([8, 24], F32)
            Bcs = newbuf("csort")
            cexp = A.alloc([8, 16], F32)
            tks = A.alloc([8, 8], F32)
            Btks = newbuf("tks")
            gate_r = ring(1, [1024], F32, "gate")
            p32 = ring(1, [256], F32, "p32")
            p16 = ring(1, [256], BF16, "p16")
            pT = A.alloc([2, 512], BF16)
            BpT = newbuf("pT")
            ot_r = ring(1, [1024], F32, "ot")

            AB = (0, 1)
            GB = ((2, 3), (4, 5))
            OBK = (6, 7)
            TP2 = 2

            for q in range(16):
                sa, sb_ = sk32.next()
                P.dma(sa, sk_d[q], W=[sb_], sbuf=sb_)
                ka, kb_ = sk16.next()
                P.cp("pool", ka, sa, [sb_], [kb_])
                P.mm(psum_t[TP2][:, (q % 4) * 128:(q % 4 + 1) * 128], ka, ident, True, True, [kb_] + CONST, [PB[TP2]])
                if q % 4 == 3:
                    P.cp("act", skT[:, q - 3:q + 1, :], psum_t[TP2][:, :].rearrange("p (k e) -> p k e", k=4),
                         [PB[TP2]], [BskT])

            def norm_T(src_ap, src_bufs, dstT, dstT_buf, j, eng_st=st2):
                sa, sbf = eng_st.next()
                P.act(junk2, src_ap, AF.Square, src_bufs, [sbf], accum=sa[:, 0:1])
                P.rstd(sa[:, 1:2], sa[:, 0:1], 1024, [sbf], [sbf])
                na, nb = xn16b.next()
                P.ts("dve", na, src_ap, sa[:, 1:2], ALU.mult, src_bufs + [sbf], [nb])
                for half in range(2):
                    tb = AB[half]
                    for kk in range(4):
                        K = half * 4 + kk
                        P.mm(psum_t[tb][:, kk * 128:(kk + 1) * 128], na[:, K * 128:(K + 1) * 128], ident, True, True,
                             [nb] + CONST, [PB[tb]])
                    P.cp("act" if half == 0 else "dve", dstT[:, half * 4:(half + 1) * 4, j * 128:(j + 1) * 128],
                         psum_t[tb][:, :].rearrange("p (k e) -> p k e", k=4), [PB[tb]], [dstT_buf])

            for pb in range(NS):
                for j in range(4):
                    row = (pb * 4 + j) * 128
                    P.dma(acc[:, j, :], h1_d[row:row + 128, :], R=[Bh1d], W=[Bacc[j]], sbuf=Bacc[j])
                if pb == 0:
                    P.dma(Wq, wq16_d.rearrange("(k p) e -> p k e", p=128), R=[Bwq16d], W=BWq, sbuf=BWq[0])
                for j in range(4):
                    norm_T(acc[:, j, :], [Bacc[j]], xnT2, BxnT2, j)
                for q in range(16):
                    h, half = q // 2, q % 2
                    ab = AB[q % 2]
                    for K in range(8):
                        P.mm(psum_t[ab][:, :], Wq[:, K, q * 128:(q + 1) * 128], xnT2[:, K, :], K == 0, K == 7,
                             BWq + [BxnT2], [PB[ab]])
                    qa, qb = qTc.next()
                    P.cp("act", qa, psum_t[ab][:, :], [PB[ab]], [qb])
                    gb = GB[q % 2][0]
                    for j in range(4):
                        P.mm(psum_t[gb][:, j * 128:(j + 1) * 128], qa[:, j * 128:(j + 1) * 128], skT[:, q, :], True, True,
                             [qb, BskT], [PB[gb]])
                    dst = (y1 if half == 0 else s2)[:, :, h, :]
                    dbufs = By1 if half == 0 else Bs2
                    P.cp("dve", dst, psum_t[gb][:, :].rearrange("p (j n) -> p j n", j=4), [PB[gb]], dbufs)
                for j in range(4):
                    for q in range(16):
                        h, half = q // 2, q % 2
                        src = (y1 if half == 0 else s2)[:, j, h, :]
                        sbuf_l = [By1[j] if half == 0 else Bs2[j]]
                        ta, tb = tk_tmp.next()
                        P.op("dve", lambda e, o=tk_v[:, q, 0:8], i=src: e.max(out=o, in_=i), sbuf_l, [Btkv])
                        P.op("dve", lambda e, o=ta, r=tk_v[:, q, 0:8], i=src: e.match_replace(
                            out=o, in_to_replace=r, in_values=i, imm_value=-1e30), sbuf_l + [Btkv], [tb])
                        P.op("dve", lambda e, o=tk_v[:, q, 8:16], i=ta: e.max(out=o, in_=i), [tb], [Btkv])
                    tv = tk_v.rearrange("p (h two) k -> p h two k", two=2)
                    P.tt("dve", cand.rearrange("p h (a b) -> p h a b", a=16),
                         tv[:, :, 0, :].unsqueeze(3).to_broadcast([128, 8, 16, 16]),
                         tv[:, :, 1, :].unsqueeze(2).to_broadcast([128, 8, 16, 16]), ALU.add, [Btkv], [Bcand])
                    for h in range(8):
                        c0 = cand[:, h, :]
                        t1, t1b = ctmp.next()
                        t2, t2b = ctmp.next()
                        P.op("dve", lambda e, o=csort[:, h, 0:8], i=c0: e.max(out=o, in_=i), [Bcand], [Bcs])
                        P.op("dve", lambda e, o=t1, r=csort[:, h, 0:8], i=c0: e.match_replace(
                            out=o, in_to_replace=r, in_values=i, imm_value=-1e30), [Bcand, Bcs], [t1b])
                        P.op("dve", lambda e, o=csort[:, h, 8:16], i=t1: e.max(out=o, in_=i), [t1b], [Bcs])
                        P.op("dve", lambda e, o=t2, r=csort[:, h, 8:16], i=t1: e.match_replace(
                            out=o, in_to_replace=r, in_values=i, imm_value=-1e30), [t1b, Bcs], [t2b])
                        P.op("dve", lambda e, o=csort[:, h, 16:24], i=t2: e.max(out=o, in_=i), [t2b], [Bcs])
                    P.tt("dve", tks[:, :, 0], csort[:, :, 15], csort[:, :, 16], ALU.add, [Bcs], [Btks])
                    P.ts("dve", tks[:, :, 0], tks[:, :, 0], 0.5, ALU.mult, [Btks], [Btks])
                    P.tt("dve", cexp, csort[:, :, 0:16], csort[:, :, 0:1].to_broadcast([128, 8, 16]), ALU.subtract,
                         [Bcs], [Btks])
                    P.act(cexp, cexp, AF.Exp, [Btks], [Btks])
                    P.op("dve", lambda e, o=tks[:, :, 1], i=cexp: e.tensor_reduce(out=o, in_=i, axis=AX.X, op=ALU.add),
                         [Btks], [Btks])
                    P.act(tks[:, :, 2], tks[:, :, 1], AF.Ln, [Btks], [Btks])
                    P.tt("dve", tks[:, :, 3], tks[:, :, 0], csort[:, :, 0], ALU.subtract, [Btks, Bcs], [Btks])
                    P.tt("dve", kap[:, j, :], tks[:, :, 3], tks[:, :, 2], ALU.subtract, [Btks], [Bkap[j]])
                    P.tt("dve", y1[:, j, :, :], y1[:, j, :, :], tks[:, :, 0:1].to_broadcast([128, 8, 128]), ALU.subtract,
                         [By1[j], Btks], [By1[j]])
                hda = hid_r.items[0][0]
                gainfo = {}

                uinfo = {}
                vinfo = {}

                def emit_u_dma(g):
                    ua, ub = ut_r.next()
                    P.dma(ua, ut_d[:, g * 1024:(g + 1) * 1024].rearrange("(k p) e -> p k e", p=128), R=[Butd], W=[ub], sbuf=ub)
                    uinfo[g] = (ua, ub)

                def emit_v_dma(g):
                    va, vb = v_r.next()
                    P.dma(va, v16_d[g * 1024:(g + 1) * 1024, :].rearrange("(c p) d -> p c d", p=128), R=[Bv16d], W=[vb], sbuf=vb)
                    vinfo[g] = (va, vb)

                def emit_a_start(g):
                    if g not in uinfo:
                        emit_u_dma(g)
                    if g not in vinfo:
                        emit_v_dma(g)
                    gaa, gab = ga_r.next()
                    gainfo[g] = (gaa, gab) + vinfo[g]

                def emit_a_mm(g, c):
                    ua, ub = uinfo[g]
                    ab = AB[c % 2]
                    for K in range(8):
                        P.mm(psum_t[ab][:, :], ua[:, K, c * 128:(c + 1) * 128], xnT2[:, K, :], K == 0, K == 7,
                             [ub, BxnT2], [PB[ab]])

                def emit_a_cp(g, c):
                    gaa, gab = gainfo[g][0:2]
                    ab = AB[c % 2]
                    P.cp("act", gaa[:, c, :], psum_t[ab][:, :], [PB[ab]], [gab])

                def emit_a_gelu(g):
                    gaa, gab = gainfo[g][0:2]
                    P.act(gaa, gaa, AF.Gelu, [gab], [gab])

                def emit_head(g, j, h):
                    gb0, gb1 = GB[j % 2]
                    da, db = D_r.next()
                    ea, eb = E_r.next()
                    P.tt("dve", da, y1[:, j, h, g * 8:(g + 1) * 8].unsqueeze(2).to_broadcast([128, 8, 128]),
                         s2[:, j, h, :].unsqueeze(1).to_broadcast([128, 8, 128]), ALU.add, [By1[j], Bs2[j]], [db])
                    if h % 2 == 0 and not (h == 6 and j % 2 == 1):
                        P.stt("dve", da, da, BIG, da, ALU.mult, ALU.min, [db], [db])
                    else:
                        P.op("act", lambda e, o=da: e.activation(out=o, in_=o, func=AF.Prelu, alpha=BIG), [db], [db])
                    P.act(ea, da, AF.Exp, [db, Bkap[j]], [eb], bias=kap[:, j, h:h + 1])
                    for c in range(GRP):
                        gbk = gb0 if c < 4 else gb1
                        P.mm(psum_t[gbk][:, (c % 4) * 128:(c % 4 + 1) * 128], ea[:, c, :], ident,
                             h == 0 and c % 4 == 0, h == 7, [eb] + CONST, [PB[gbk]], skip=True)

                def emit_hid(g, j):
                    gaa, gab, va, vb = gainfo[g]
                    for half in range(2):
                        gbk = GB[j % 2][half]
                        P.tt("dve", hda[:, half * 4:(half + 1) * 4, j * 128:(j + 1) * 128],
                             gaa[:, half * 4:(half + 1) * 4, j * 128:(j + 1) * 128],
                             psum_t[gbk][:, :].rearrange("p (c t) -> p c t", c=4), ALU.mult, [gab, PB[gbk]], [Bhid[j]])

                def emit_O(g, j):
                    gaa, gab, va, vb = gainfo[g]
                    for half in range(2):
                        ob = OBK[half]
                        for c in range(GRP):
                            P.mm(psum_t[ob][:, :], hda[:, c, j * 128:(j + 1) * 128], va[:, c, half * 512:(half + 1) * 512],
                                 c == 0, c == GRP - 1, [Bhid[j], vb], [PB[ob]])

                def emit_acc(g, j):
                    for half in range(2):
                        ob = OBK[half]
                        P.tt("dve", acc[:, j, half * 512:(half + 1) * 512], acc[:, j, half * 512:(half + 1) * 512],
                             psum_t[ob][:, :], ALU.add, [Bacc[j], PB[ob]], [Bacc[j]])

                emit_a_start(0)
                for c in range(GRP):
                    emit_a_mm(0, c)
                    emit_a_cp(0, c)
                emit_a_gelu(0)
                steps = [(g, j) for g in range(NGRP) for j in range(4)]
                prev = None
                for (g, j) in steps:
                    for h in range(8):
                        emit_head(g, j, h)
                        if prev is not None and h == 1:
                            emit_hid(*prev)
                            emit_O(*prev)
                        if prev is not None and h == 4:
                            emit_acc(*prev)
                        if j == 0 and h == 0 and g + 1 < NGRP:
                            emit_u_dma(g + 1)
                        if j == 0 and h == 3 and g + 1 < NGRP:
                            emit_v_dma(g + 1)
                        if g + 1 < NGRP:
                            if j == 1 and h == 0:
                                emit_a_start(g + 1)
                            if j in (1, 2):
                                c_ = (j - 1) * 4 + h // 2
                                if h % 2 == 0:
                                    emit_a_mm(g + 1, c_)
                                else:
                                    emit_a_cp(g + 1, c_)
                            if j == 3 and h == 3:
                                emit_a_gelu(g + 1)
                        if g == NGRP - 1 and j == 1 and h == 0 and pb + 1 < NS:
                            P.dma(Wq, wq16_d.rearrange("(k p) e -> p k e", p=128), R=[Bwq16d], W=BWq, sbuf=BWq[0])
                    prev = (g, j)
                emit_hid(*prev)
                emit_O(*prev)
                emit_acc(*prev)
                P.dma(Wg[:, :, 0:1024], wg16_d.rearrange("(k p) e -> p k e", p=128), R=[Bwg16d], W=BWg, sbuf=BWg[0])
                P.dma(Wg[:, 0:2, 1024:2048], wp16_d.rearrange("(k p) e -> p k e", p=128), R=[Bwp16d], W=BWg, sbuf=BWg[0])
                for j in range(4):
                    norm_T(acc[:, j, :], [Bacc[j]], xnT2, BxnT2, j)
                for j in range(4):
                    row = (pb * 4 + j) * 128
                    pa, pb_ = p32.next()
                    P.dma(pa, po_d[row:row + 128, :], W=[pb_], sbuf=pb_)
                    p6, p6b = p16.next()
                    P.cp("pool", p6, pa, [pb_], [p6b])
                    for K in range(2):
                        P.mm(psum_t[GB[0][0]][:, K * 128:(K + 1) * 128], p6[:, K * 128:(K + 1) * 128], ident, True, True,
                             [p6b] + CONST, [PB[GB[0][0]]])
                    P.cp("act", pT[:, :, j * 128:(j + 1) * 128],
                         psum_t[GB[0][0]][:, 0:256].rearrange("p (k e) -> p k e", k=2), [PB[GB[0][0]]], [BpT])
                for j in range(4):
                    ga_, gb_ = gate_r.next()
                    oa, ob_ = ot_r.next()
                    for half in range(2):
                        ab = AB[half]
                        for K in range(8):
                            P.mm(psum_t[ab][:, :], xnT2[:, K, j * 128:(j + 1) * 128], Wg[:, K, half * 512:(half + 1) * 512],
                                 K == 0, K == 7, [BxnT2] + BWg, [PB[ab]])
                        P.act(ga_[:, half * 512:(half + 1) * 512], psum_t[ab][:, :], AF.Sigmoid, [PB[ab]], [gb_])
                        ob = OBK[half]
                        for K in range(2):
                            P.mm(psum_t[ob][:, :], pT[:, K, j * 128:(j + 1) * 128],
                                 Wg[:, K, 1024 + half * 512:1024 + (half + 1) * 512], K == 0, K == 1, [BpT] + BWg, [PB[ob]])
                        P.tt("dve", ga_[:, half * 512:(half + 1) * 512], ga_[:, half * 512:(half + 1) * 512], psum_t[ob][:, :],
                             ALU.mult, [gb_, PB[ob]], [gb_])
                        P.tt("pool", oa[:, half * 512:(half + 1) * 512], ga_[:, half * 512:(half + 1) * 512],
                             acc[:, j, half * 512:(half + 1) * 512], ALU.add, [gb_, Bacc[j]], [ob_])
                    row = (pb * 4 + j) * 128
                    P.dma(out_d[row:row + 128, :], oa, R=[ob_], sbuf=ob_)
                final_bufs = ot_r.bufs()
        if debug and 2 not in phases:
            A.reset(persist_off)
            da_, db_ = A.alloc([1024], F32), newbuf("dbg")
            for r0 in range(0, NO, 128):
                P.dma(da_, h1_d[r0:r0 + 128, :], R=[Bh1d], W=[db_], sbuf=db_)
                P.dma(dbg_d[r0:r0 + 128, :], da_, R=[db_], sbuf=db_)
            final_bufs = [db_]
        A.reset(A.off)
        print('arena high-water words', A.hw, 'of', AW)
        P.emit(final_bufs=list(final_bufs) + dumps)
    return nc


POOL_WINDOWS = (2, 4, 8, 16)


def own_blocks(r, nblk):
    out = []
    for s in range(nblk // 2):
        lo = (s % 2 == 0)
        if r == 0:
            out.append(2 * s if lo else 2 * s + 1)
        else:
            out.append(2 * s + 1 if lo else 2 * s)
    return out


def host_consts(S):
    bf = ml_dtypes.bfloat16
    NS = S // 1024
    t = np.arange(128)
    bgen = np.zeros((128, 4, 2, 128), np.float32)
    for g, w in enumerate(POOL_WINDOWS):
        cur = ((t[:, None] <= t[None, :]) & (t[:, None] > t[None, :] - w)).astype(np.float32) / w - np.eye(128, dtype=np.float32)
        prev = ((t[:, None] - 128 > t[None, :] - w)).astype(np.float32) / w
        bgen[:, g, 0, :] = prev
        bgen[:, g, 1, :] = cur
    return bgen.astype(bf)


def core_inputs(b, r, S, x, p, positions, shared):
    bf = ml_dtypes.bfloat16
    NS = S // 1024
    nblk = S // 512
    blocks = own_blocks(r, nblk)
    xs = np.ascontiguousarray(x[b])
    pos = positions[b].astype(np.int32)
    own_idx = np.concatenate([np.arange(k * 512, (k + 1) * 512) for k in blocks])
    xo = np.ascontiguousarray(xs[own_idx])
    po = np.ascontiguousarray(p[0, b][own_idx])
    xh = np.zeros((NS * 16, D), np.float32)
    t = np.arange(128)
    bfirst = np.zeros((NS, 128, 4, 128), np.float32)
    amask = np.zeros((NS, 8, 128, 512), np.float32)
    for s, k in enumerate(blocks):
        if k > 0:
            xh[s * 16:(s + 1) * 16] = xs[k * 512 - 16:k * 512]
        for g, w in enumerate(POOL_WINDOWS):
            band = ((t[:, None] <= t[None, :]) & (t[:, None] > t[None, :] - w)).astype(np.float32)
            if k == 0:
                cnt = np.minimum(t + 1, w).astype(np.float32)
                bfirst[s, :, g, :] = band / cnt[None, :] - np.eye(128, dtype=np.float32)
            else:
                bfirst[s, :, g, :] = band / w - np.eye(128, dtype=np.float32)
        qpos = k * 512 + np.arange(512)
        for kk in range(8):
            kpos = (2 * s) * 512 + kk * 128 + np.arange(128)
            amask[s, kk] = (kpos[:, None] <= qpos[None, :]).astype(np.float32)
    d = dict(shared)
    d.update(
        xs=xs, pos_s=np.ascontiguousarray(pos.reshape(S // 128, 128).T),
        xo=xo, pos_o=np.ascontiguousarray(pos[own_idx].reshape(NS * 4, 128).T),
        xh=xh, amask=amask.astype(bf), bfirst=bfirst.astype(bf), po=po,
    )
    return d, own_idx


def shared_inputs(S, ln_mix, w_in, pool_w, pool_scale, q_norm, k_norm, lambda_q1, lambda_k1, lambda_q2, lambda_k2,
                  subln, w_o, ln_ffn, w_peer_q, peer_subkeys, peer_u, peer_v, ln_pe, w_pe_gate, w_pe_proj):
    f = lambda a: np.ascontiguousarray(np.asarray(a, np.float32))
    colT = lambda v: f(np.asarray(v, np.float32).reshape(-1, 128).T)
    rep = lambda v: f(np.broadcast_to(np.asarray(v, np.float32), (128,) + np.asarray(v).shape))
    rowsc = np.concatenate([colT(pool_scale[0]), np.asarray(subln[0], np.float32).reshape(128, 1).repeat(4, axis=1)], axis=1)
    inv_freq = (10000.0 ** (-np.arange(0, 64, 2, dtype=np.float32) / 64)).astype(np.float32)
    return dict(
        bgen=host_consts(S),
        ln_mix_t=colT(ln_mix[0]), w_in=f(w_in[0]), pool_w=f(pool_w[0]), rowscale_t=f(rowsc),
        qk_gain=f(np.stack([rep(q_norm[0]), rep(k_norm[0])], axis=1)),
        lam_vecs=f(np.stack([rep(lambda_q1[0]), rep(lambda_k1[0]), rep(lambda_q2[0]), rep(lambda_k2[0])], axis=1)),
        w_o=f(w_o[0]), ln_ffn_t=colT(ln_ffn[0]), w_peer_q=f(w_peer_q[0]),
        peer_subkeys=f(np.asarray(peer_subkeys[0], np.float32).reshape(16, 128, 128)),
        peer_u=f(peer_u[0]), peer_v=f(peer_v[0]), ln_pe_t=colT(ln_pe[0]), w_pe_gate=f(w_pe_gate[0]),
        w_pe_proj=f(w_pe_proj[0]), ident=np.eye(128, dtype=np.float32), invfreq=rep(inv_freq),
    )


def run(x, p, positions, weights, phases=(0, 1, 2), debug=False):
    x = np.asarray(x, np.float32)
    p = np.asarray(p, np.float32)
    positions = np.asarray(positions)
    B, S, _ = x.shape
    shared = shared_inputs(S, **weights)
    nc = build(S, phases=phases, debug=debug)
    in_maps, idxs = [], []
    for b in range(B):
        for r in range(2):
            d, own_idx = core_inputs(b, r, S, x, p, positions, shared)
            in_maps.append(d)
            idxs.append((b, own_idx))
    res = run_bass_kernel_spmd(nc, in_maps, core_ids=list(range(2 * B)))
    out = np.zeros((B, S, D), np.float32)
    key = "dbg" if (debug and 2 not in phases) else "out"
    for (b, own_idx), r in zip(idxs, res.results):
        out[b, own_idx] = r[key]
    if debug:
        return out, res.results
    return out


def kernel(x, p, positions, **weights):
    return run(x, p, positions, weights)
```

```python
import math
import contextlib
import numpy as np
import ml_dtypes
import concourse.bass as bass
import concourse.mybir as mybir
from concourse.bass_utils import run_bass_kernel_spmd

F32 = mybir.dt.float32
BF16 = mybir.dt.bfloat16
I32 = mybir.dt.int32
ALU = mybir.AluOpType
AF = mybir.ActivationFunctionType
AX = mybir.AxisListType
COMPUTE = ("pe", "act", "dve", "pool")
EPS = 1e-6
PI = math.pi


class Buf:
    __slots__ = ("name", "last_w", "readers", "dsem", "dcount")

    def __init__(self, name=""):
        self.name = name
        self.last_w = None
        self.readers = []
        self.dsem = None
        self.dcount = 0


class Instr:
    __slots__ = ("eng", "fn", "deps", "is_dma", "dbuf", "dtarget", "signal", "sigval")

    def __init__(self, eng, fn, is_dma=False):
        self.eng = eng
        self.fn = fn
        self.deps = []
        self.is_dma = is_dma
        self.dbuf = None
        self.dtarget = 0
        self.signal = False
        self.sigval = 0


class Prog:
    def __init__(self, nc):
        self.nc = nc
        self.streams = {e: [] for e in ("pe", "act", "dve", "pool", "sp")}
        self.dma_bufs = []

    def _dep_on(self, ins, prev, kind):
        if prev is None or prev is ins:
            return
        if prev.is_dma:
            ins.deps.append(("d", prev.dbuf, prev.dtarget))
            return
        if prev.eng == ins.eng and not ins.is_dma:
            if ins.eng == "pe" or kind == "war":
                return
        ins.deps.append(("c", prev))

    def _track(self, ins, reads, writes):
        for b in reads:
            self._dep_on(ins, b.last_w, "raw")
        for b in writes:
            self._dep_on(ins, b.last_w, "waw")
            for r in b.readers:
                self._dep_on(ins, r, "war")
        for b in writes:
            b.last_w = ins
            b.readers = []
        for b in reads:
            if b.last_w is not ins:
                b.readers.append(ins)

    def op(self, eng, fn, R=(), W=()):
        ins = Instr(eng, fn)
        self._track(ins, R, W)
        self.streams[eng].append(ins)
        return ins

    def dma(self, out, in_, R=(), W=(), sbuf=None, queue="sp"):
        ins = Instr(queue, lambda e: e.dma_start(out=out, in_=in_), is_dma=True)
        if sbuf.dsem is None:
            sbuf.dsem = len(self.dma_bufs)
            self.dma_bufs.append(sbuf)
        self._track(ins, R, W)
        sbuf.dcount += 16
        ins.dbuf = sbuf
        ins.dtarget = sbuf.dcount
        self.streams[queue].append(ins)
        return ins

    def barrier(self, all_bufs):
        lasts = [self.streams[e][-1] for e in COMPUTE if self.streams[e]]
        for e in ("pe", "act", "dve", "pool", "sp"):
            ins = Instr(e, None)
            for p in lasts:
                if p.eng != e:
                    ins.deps.append(("c", p))
            for b in self.dma_bufs:
                if b.dcount:
                    ins.deps.append(("d", b, b.dcount))
            self.streams[e].append(ins)
        for b in all_bufs:
            b.last_w = None
            b.readers = []

    def mm(self, out, lhsT, rhs, start, stop, R, W, skip=False):
        if skip:
            return self.op("pe", lambda e: e.matmul(out, lhsT, rhs, start=start, stop=stop, skip_group_check=True), R, W)
        return self.op("pe", lambda e: e.matmul(out, lhsT, rhs, start=start, stop=stop), R, W)

    def act(self, out, in_, func, R, W, bias=None, scale=None, accum=None):
        kw = {}
        if bias is not None:
            kw["bias"] = bias
        if scale is not None:
            kw["scale"] = scale
        if accum is not None:
            kw["accum_out"] = accum
        return self.op("act", lambda e: e.activation(out=out, in_=in_, func=func, **kw), R, W)

    def tt(self, eng, out, in0, in1, op, R, W):
        return self.op(eng, lambda e: e.tensor_tensor(out=out, in0=in0, in1=in1, op=op), R, W)

    def ts(self, eng, out, in0, s1, op0, R, W, s2=None, op1=None):
        if op1 is None:
            return self.op(eng, lambda e: e.tensor_scalar(out=out, in0=in0, scalar1=s1, scalar2=None, op0=op0), R, W)
        return self.op(eng, lambda e: e.tensor_scalar(out=out, in0=in0, scalar1=s1, scalar2=s2, op0=op0, op1=op1), R, W)

    def stt(self, eng, out, in0, scalar, in1, op0, op1, R, W):
        return self.op(eng, lambda e: e.scalar_tensor_tensor(out=out, in0=in0, scalar=scalar, in1=in1, op0=op0, op1=op1), R, W)

    def cp(self, eng, out, in_, R, W):
        if eng == "act":
            return self.op("act", lambda e: e.copy(out=out, in_=in_), R, W)
        return self.op(eng, lambda e: e.tensor_copy(out=out, in_=in_), R, W)

    def recip(self, out, in_, R, W):
        return self.op("dve", lambda e: e.reciprocal(out=out, in_=in_), R, W)

    def memset(self, eng, ap, val, W):
        return self.op(eng, lambda e: e.memset(ap, val), (), W)

    def rstd(self, rs, ms, count, R, W):
        n = int(rs.shape[-1])
        self.ts("pool", rs, ms, 1.0 / count, ALU.mult, R, W, s2=EPS, op1=ALU.add)
        self.tt("pool", rs, rs, self.mhalf[:, 0:n], ALU.pow, W, W)

    def emit(self, final_bufs=()):
        nc = self.nc
        for lst in self.streams.values():
            for ins in lst:
                for d in ins.deps:
                    if d[0] == "c":
                        d[1].signal = True
        for e in COMPUTE:
            c = 0
            for ins in self.streams[e]:
                if ins.signal:
                    c += 1
                ins.sigval = c
        with contextlib.ExitStack() as st:
            csem = {e: st.enter_context(nc.semaphore("cs_" + e)) for e in COMPUTE}
            dsem = [st.enter_context(nc.semaphore("ds%d" % i)) for i in range(len(self.dma_bufs))]
            block = st.enter_context(nc.Block())
            engobj = {"pe": "tensor", "act": "scalar", "dve": "vector", "pool": "gpsimd", "sp": "sync"}

            def make(e):
                lst = self.streams[e]

                def body(eng):
                    seen_c = {x: 0 for x in COMPUTE}
                    seen_d = {}
                    for ins in lst:
                        need_c = {}
                        need_d = {}
                        for d in ins.deps:
                            if d[0] == "c":
                                p = d[1]
                                if p.sigval > need_c.get(p.eng, 0):
                                    need_c[p.eng] = p.sigval
                            else:
                                b, t = d[1], d[2]
                                if t > need_d.get(b.dsem, 0):
                                    need_d[b.dsem] = t
                        for pe_, v in need_c.items():
                            if v > seen_c[pe_]:
                                eng.wait_ge(csem[pe_], v)
                                seen_c[pe_] = v
                        for si, v in need_d.items():
                            if v > seen_d.get(si, 0):
                                eng.wait_ge(dsem[si], v)
                                seen_d[si] = v
                        if ins.fn is None:
                            continue
                        bi = ins.fn(eng)
                        if ins.is_dma:
                            bi.then_inc(dsem[ins.dbuf.dsem], 16)
                        elif ins.signal:
                            bi.then_inc(csem[e], 1)
                    if e == "sp":
                        for b in final_bufs:
                            eng.wait_ge(dsem[b.dsem], b.dcount)
                return body

            for e in ("sp", "pe", "act", "dve", "pool"):
                getattr(block, engobj[e])(make(e))


class Arena:
    def __init__(self, ap, words):
        self.ap = ap
        self.words = words
        self.off = 0

    def reset(self, off=0):
        self.hw = max(getattr(self, "hw", 0), self.off)
        self.off = off

    def alloc(self, free_shape, dtype):
        n = 1
        for s in free_shape:
            n *= s
        words = n if dtype in (F32, I32) else (n + 1) // 2
        assert self.off + words <= self.words, ("arena overflow", self.off, words, self.words)
        v = self.ap[:, self.off:self.off + words]
        self.off += words
        if dtype != F32:
            v = v.bitcast(dtype)
            if v.shape[1] != n:
                v = v[:, 0:n]
        if len(free_shape) > 1:
            names = ["a%d" % i for i in range(len(free_shape))]
            v = v.rearrange("p (%s) -> p %s" % (" ".join(names), " ".join(names)),
                            **{names[i]: free_shape[i] for i in range(len(free_shape))})
        return v


class Ring:
    def __init__(self, arena, n, free_shape, dtype, name):
        self.items = [(arena.alloc(free_shape, dtype), Buf("%s%d" % (name, i))) for i in range(n)]
        self.i = 0

    def next(self):
        it = self.items[self.i % len(self.items)]
        self.i += 1
        return it

    def bufs(self):
        return [b for _, b in self.items]


D = 1024
NEXP = 16384
NCH = 128
GRP = 8
NGRP = NCH // GRP
BIG = 1.0e6


def build(S, phases=(0, 1, 2), debug=False):
    NS = S // 1024
    NO = NS * 512
    NT = S // 128
    nc = bass.Bass("TRN2", target_bir_lowering=False)

    def din(name, shape, dt=F32):
        return nc.dram_tensor(name, shape, dt, kind="ExternalInput").ap()

    xs_d = din("xs", [S, D])
    pos_s_d = din("pos_s", [128, NT], I32)
    xo_d = din("xo", [NO, D])
    pos_o_d = din("pos_o", [128, NS * 4], I32)
    xh_d = din("xh", [NS * 16, D])
    amask_d = din("amask", [NS, 8, 128, 512], BF16)
    bgen_d = din("bgen", [128, 4, 2, 128], BF16)
    bfirst_d = din("bfirst", [NS, 128, 4, 128], BF16)
    po_d = din("po", [NO, 256])
    lnmix_d = din("ln_mix_t", [128, 8])
    w_in_d = din("w_in", [D, 2048])
    poolw_d = din("pool_w", [4, 128, 128])
    rowsc_d = din("rowscale_t", [128, 8])
    qkg_d = din("qk_gain", [128, 2, 64])
    lam_d = din("lam_vecs", [128, 4, 64])
    w_o_d = din("w_o", [D, D])
    lnffn_d = din("ln_ffn_t", [128, 8])
    wq_d = din("w_peer_q", [D, 2048])
    sk_d = din("peer_subkeys", [16, 128, 128])
    u_d = din("peer_u", [NEXP, D])
    v_d = din("peer_v", [NEXP, D])
    lnpe_d = din("ln_pe_t", [128, 8])
    wg_d = din("w_pe_gate", [D, D])
    wp_d = din("w_pe_proj", [256, D])
    ident_d = din("ident", [128, 128])
    invf_d = din("invfreq", [128, 32])
    out_d = nc.dram_tensor("out", [NO, D], F32, kind="ExternalOutput").ap()
    dbg_d = nc.dram_tensor("dbg", [NO, D], F32, kind="ExternalOutput").ap() if debug else None

    def dscr(name, shape, dt):
        return nc.dram_tensor(name, shape, dt, kind="Internal").ap()

    h1_d = dscr("h1s", [NO, D], F32)
    ut_d = dscr("ut16", [D, NEXP], BF16)
    v16_d = dscr("v16", [NEXP, D], BF16)
    wq16_d = dscr("wq16", [D, 2048], BF16)
    wg16_d = dscr("wg16", [D, D], BF16)
    wp16_d = dscr("wp16", [256, D], BF16)
    Bh1d, Butd, Bv16d, Bwq16d, Bwg16d, Bwp16d = [Buf(n) for n in ("h1d", "utd", "v16d", "wq16d", "wg16d", "wp16d")]

    AW = 53100
    with contextlib.ExitStack() as st:
        arena_t = st.enter_context(nc.sbuf_tensor("arena", [128, AW], F32))
        psum_t = [st.enter_context(nc.psum_tensor("pb%d" % i, [128, 512], F32)) for i in range(8)]
        PB = [Buf("psum%d" % i) for i in range(8)]
        P = Prog(nc)
        A = Arena(arena_t[:, :], AW)
        allbufs = list(PB)

        dumps = []

        def dump(name, ap, R):
            if not debug:
                return
            shp = [int(x) for x in ap.shape]
            dd = nc.dram_tensor("dump_" + name, shp, ap.dtype, kind="ExternalOutput").ap()
            b = Buf("dump_" + name)
            P.dma(dd, ap, R=R, sbuf=b)
            dumps.append(b)

        def newbuf(name):
            b = Buf(name)
            allbufs.append(b)
            return b

        def ring(n, shape, dt, name):
            r = Ring(A, n, shape, dt, name)
            allbufs.extend(r.bufs())
            return r

        Bc = newbuf("consts")
        ident = A.alloc([128], BF16)
        lnmix = A.alloc([8], F32)
        rowsc = A.alloc([8], F32)
        lnffn = A.alloc([8], F32)
        lnpe = A.alloc([8], F32)
        qkg = A.alloc([2, 64], F32)
        invf = A.alloc([32], F32)
        posf_s = A.alloc([NT], F32)
        posf_o = A.alloc([NS * 4], F32)
        poolw = A.alloc([4, 128], BF16)
        bgen = A.alloc([4, 2, 128], BF16)
        nlam = A.alloc([1], F32)
        mhalf = A.alloc([8], F32)
        P.mhalf = mhalf
        persist_off = A.off
        ident32 = A.alloc([128], F32)
        lamv = A.alloc([4, 64], F32)
        posi_s = A.alloc([NT], I32)
        posi_o = A.alloc([NS * 4], I32)
        poolw32 = A.alloc([4, 128], F32)
        lam_s = A.alloc([8], F32)
        for dst, src in ((ident32, ident_d), (lnmix, lnmix_d), (rowsc, rowsc_d), (lnffn, lnffn_d), (lnpe, lnpe_d),
                         (qkg, qkg_d), (lamv, lam_d), (invf, invf_d), (posi_s, pos_s_d), (posi_o, pos_o_d),
                         (bgen, bgen_d)):
            P.dma(dst, src, W=[Bc], sbuf=Bc)
        P.dma(poolw32, poolw_d.rearrange("g c e -> c g e"), W=[Bc], sbuf=Bc)
        Bk = newbuf("consts2")
        P.cp("dve", ident, ident32, [Bc], [Bk])
        P.memset("dve", mhalf, -0.5, [Bk])
        P.cp("dve", poolw, poolw32, [Bc], [Bk])
        P.cp("dve", posf_s, posi_s, [Bc], [Bk])
        P.cp("dve", posf_o, posi_o, [Bc], [Bk])
        lam_init = 0.8 - 0.6 * math.exp(-0.3 * 0)
        lprod = A.alloc([2, 64], F32)
        P.tt("dve", lprod[:, 0, :], lamv[:, 0, :], lamv[:, 1, :], ALU.mult, [Bc], [Bk])
        P.tt("dve", lprod[:, 1, :], lamv[:, 2, :], lamv[:, 3, :], ALU.mult, [Bc], [Bk])
        P.op("dve", lambda e: e.tensor_reduce(out=lam_s[:, 0:2], in_=lprod, axis=AX.X, op=ALU.add), [Bk], [Bk])
        P.act(lam_s[:, 2:4], lam_s[:, 0:2], AF.Exp, [Bk], [Bk])
        P.tt("dve", lam_s[:, 4:5], lam_s[:, 3:4], lam_s[:, 2:3], ALU.subtract, [Bk], [Bk])
        P.ts("dve", nlam, lam_s[:, 4:5], -lam_init, ALU.add, [Bk], [Bk])
        P.ts("dve", rowsc[:, 4:8], rowsc[:, 4:8], 1.0 - lam_init, ALU.mult, [Bc], [Bk])
        P.barrier(allbufs)
        A.reset(persist_off)
        CONST = []

        w_in16 = A.alloc([8, 2048], BF16)
        w_o16 = A.alloc([8, 1024], BF16)
        Bwin, Bwo = newbuf("w_in16"), newbuf("w_o16")
        mixer_off = A.off

        if 0 in phases:
            A.reset(mixer_off)
            stage8 = ring(2, [2048], F32, "stage8")
            stage4 = ring(4, [1024], F32, "stage4")
            cast4 = ring(3, [1024], BF16, "cast4")
            utg = ring(2, [8, 1024], BF16, "utg")
            w16s = ring(2, [2048], BF16, "w16s")
            engs2 = ("dve", "pool")
            for K in range(8):
                sa, sbuf_ = stage8.next()
                P.dma(sa, w_in_d[K * 128:(K + 1) * 128, :], W=[sbuf_], sbuf=sbuf_)
                P.ts(engs2[K % 2], w_in16[:, K, :], sa, lnmix[:, K:K + 1], ALU.mult, [sbuf_] + CONST, [Bwin])
            for K in range(8):
                sa, sbuf_ = stage4.next()
                P.dma(sa, w_o_d[K * 128:(K + 1) * 128, :], W=[sbuf_], sbuf=sbuf_)
                P.ts(engs2[K % 2], w_o16[:, K, :], sa, rowsc[:, K:K + 1], ALU.mult, [sbuf_] + CONST, [Bwo])
            for K in range(8):
                sa, sbuf_ = stage8.next()
                P.dma(sa, wq_d[K * 128:(K + 1) * 128, :], W=[sbuf_], sbuf=sbuf_)
                wa, wb = w16s.next()
                P.ts(engs2[K % 2], wa, sa, lnffn[:, K:K + 1], ALU.mult, [sbuf_] + CONST, [wb])
                P.dma(wq16_d[K * 128:(K + 1) * 128, :], wa, R=[wb], W=[Bwq16d], sbuf=wb)
            for K in range(8):
                sa, sbuf_ = stage4.next()
                P.dma(sa, wg_d[K * 128:(K + 1) * 128, :], W=[sbuf_], sbuf=sbuf_)
                wa, wb = cast4.next()
                P.ts(engs2[K % 2], wa, sa, lnpe[:, K:K + 1], ALU.mult, [sbuf_] + CONST, [wb])
                P.dma(wg16_d[K * 128:(K + 1) * 128, :], wa, R=[wb], W=[Bwg16d], sbuf=wb)
            for K in range(2):
                sa, sbuf_ = stage4.next()
                P.dma(sa, wp_d[K * 128:(K + 1) * 128, :], W=[sbuf_], sbuf=sbuf_)
                wa, wb = cast4.next()
                P.cp(engs2[K % 2], wa, sa, [sbuf_], [wb])
                P.dma(wp16_d[K * 128:(K + 1) * 128, :], wa, R=[wb], W=[Bwp16d], sbuf=wb)
            cengs = ("act", "pool", "dve")
            for i in range(NCH):
                sa, sbuf_ = stage4.next()
                P.dma(sa, v_d[i * 128:(i + 1) * 128, :], W=[sbuf_], sbuf=sbuf_)
                wa, wb = cast4.next()
                P.cp(cengs[i % 3], wa, sa, [sbuf_], [wb])
                P.dma(v16_d[i * 128:(i + 1) * 128, :], wa, R=[wb], W=[Bv16d], sbuf=wb)
            for g in range(NGRP):
                ua, ub = utg.next()
                for c in range(GRP):
                    i = g * GRP + c
                    sa, sbuf_ = stage4.next()
                    P.dma(sa, u_d[i * 128:(i + 1) * 128, :], W=[sbuf_], sbuf=sbuf_)
                    wa, wb = cast4.next()
                    P.cp(cengs[i % 3], wa, sa, [sbuf_], [wb])
                    for half in range(2):
                        pbi = (i * 2 + half) % 4
                        for kk in range(4):
                            K = half * 4 + kk
                            P.mm(psum_t[pbi][:, kk * 128:(kk + 1) * 128], wa[:, K * 128:(K + 1) * 128], ident,
                                 True, True, [wb] + CONST, [PB[pbi]])
                        P.cp("act" if half == 0 else "dve", ua[:, half * 4:(half + 1) * 4, c * 128:(c + 1) * 128],
                             psum_t[pbi][:, :].rearrange("p (k e) -> p k e", k=4), [PB[pbi]], [ub])
                P.dma(ut_d[:, g * 1024:(g + 1) * 1024].rearrange("(k p) e -> p k e", p=128), ua, R=[ub], W=[Butd], sbuf=ub)
            P.barrier(allbufs)

        if 1 in phases:
            A.reset(mixer_off)
            KT = A.alloc([2, S], BF16)
            Vp = A.alloc([NT, 2, 130], BF16)
            yT01 = A.alloc([2, NO], BF16)
            BKT = [newbuf("KT%d" % i) for i in range(S // 512)]
            BV = [newbuf("V%d" % i) for i in range(S // 512)]
            ByT01 = [newbuf("yT01_%d" % i) for i in range(NS)]
            Bones = newbuf("vones")
            xt = ring(2, [1024], F32, "xt")
            junk = A.alloc([1024], BF16)
            Bjunk = newbuf("junk")
            xn16 = ring(2, [1024], BF16, "xn16")
            st_r = ring(4, [8], F32, "stat")
            st_n = ring(3, [8], F32, "stat_n")
            st_q = ring(3, [8], F32, "stat_q")
            xnT = ring(2, [8, 512], BF16, "xnT")
            qk32 = ring(2, [256], F32, "qk32")
            qkA = ring(2, [256], F32, "qkA")
            qkB = ring(2, [128], F32, "qkB")
            qk16 = ring(2, [2, 128], BF16, "qk16")
            cs_r = ring(1, [2, 4, 32], F32, "cossin")
            cs_tmp = ring(1, [4, 32], F32, "cstmp")
            cs_tmi = A.alloc([4, 32], I32)
            Bcsi = newbuf("cstmi")
            QT = ring(1, [2, 512], BF16, "QT")
            pt_r = ring(4, [512], BF16, "pt")
            msk_r = ring(1, [8, 512], BF16, "msk")
            o_r = ring(2, [128], F32, "o32")
            y16_r = ring(2, [128], BF16, "y16")
            zt = A.alloc([5, 512], BF16)
            Bzt = newbuf("zt")
            mT = ring(2, [512], BF16, "mT")
            yTb = A.alloc([6, 512], BF16)
            ByTb = newbuf("yTb")
            bfirst = A.alloc([4, 128], BF16)
            Bbf = newbuf("bfirst")
            h1t = ring(1, [1024], F32, "h1t")
            haloT = A.alloc([8, 128], BF16)
            BhaloT = newbuf("haloT")
            ZB = (0, 1)
            TPB = 2
            SB_ = (3, 4)
            OB = (5, 6, 7)
            O_SLOT = [(OB[i // 3], (i % 3) * 160) for i in range(8)]

            P.memset("pool", Vp[:, :, :, 128:130], 1.0, [Bones])

            def nt_load(src_d, row0):
                xa, xb = xt.next()
                P.dma(xa, src_d[row0:row0 + 128, :], W=[xb], sbuf=xb)
                return xa, xb

            def nt_compute(ld, xnT_ap, xnT_buf, j):
                xa, xb = ld
                sa, sbf = st_n.next()
                P.act(junk, xa, AF.Square, [xb], [sbf], accum=sa[:, 0:1])
                yield
                P.rstd(sa[:, 1:2], sa[:, 0:1], 1024, [sbf], [sbf])
                na, nb = xn16.next()
                P.ts("dve", na, xa, sa[:, 1:2], ALU.mult, [xb, sbf], [nb])
                yield
                for half in range(2):
                    for kk in range(4):
                        K = half * 4 + kk
                        P.mm(psum_t[TPB][:, kk * 128:(kk + 1) * 128], na[:, K * 128:(K + 1) * 128], ident, True, True,
                             [nb] + CONST, [PB[TPB]])
                    P.cp("act" if half == 0 else "dve", xnT_ap[:, half * 4:(half + 1) * 4, j * 128:(j + 1) * 128],
                         psum_t[TPB][:, :].rearrange("p (k e) -> p k e", k=4), [PB[TPB]], [xnT_buf])
                yield

            def norm_block(src_d, tile0, xnT_ap, xnT_buf):
                lds = [nt_load(src_d, tile0 * 128), nt_load(src_d, (tile0 + 1) * 128)]
                for j in range(4):
                    yield from nt_compute(lds[j], xnT_ap, xnT_buf, j)
                    if j + 2 < 4:
                        lds.append(nt_load(src_d, (tile0 + j + 2) * 128))

            def cos_sin(posf, col0):
                ca, cb = cs_r.next()
                ta, tb = cs_tmp.next()
                for which, shift in ((1, 0.0), (0, PI / 2)):
                    dst = ca[:, which, :, :]
                    P.tt("dve", dst, posf[:, col0:col0 + 4].unsqueeze(2).to_broadcast([128, 4, 32]),
                         invf.unsqueeze(1).to_broadcast([128, 4, 32]), ALU.mult, CONST, [cb])
                    if shift:
                        P.ts("dve", dst, dst, shift, ALU.add, [cb], [cb])
                    P.ts("dve", ta, dst, 1.0 / (2 * PI), ALU.mult, [cb], [tb])
                    P.cp("dve", cs_tmi, ta, [tb], [Bcsi])
                    P.cp("dve", ta, cs_tmi, [Bcsi], [tb])
                    P.stt("dve", dst, ta, -2 * PI, dst, ALU.mult, ALU.add, [tb, cb], [cb])
                    P.ts("dve", ta, dst, PI, ALU.is_gt, [cb], [tb], s2=-2 * PI, op1=ALU.mult)
                    P.tt("dve", dst, dst, ta, ALU.add, [cb, tb], [cb])
                    P.ts("dve", ta, dst, -PI, ALU.is_lt, [cb], [tb], s2=2 * PI, op1=ALU.mult)
                    P.tt("dve", dst, dst, ta, ALU.add, [cb, tb], [cb])
                P.act(ca, ca, AF.Sin, [cb], [cb])
                return ca, cb

            def qk_norm_rope(zps, zbuf, gi, ca, cb, j):
                sa, sbf = st_r.next()
                q32, q32b = qk32.next()
                P.act(q32, zps, AF.Square, [zbuf], [q32b])
                P.op("dve", lambda e: e.tensor_reduce(out=sa[:, 0:4], in_=q32.rearrange("p (a d) -> p a d", a=4),
                                                      axis=AX.X, op=ALU.add), [q32b], [sbf])
                P.rstd(sa[:, 4:8], sa[:, 0:4], 64, [sbf], [sbf])
                P.tt("dve", q32.rearrange("p (a d) -> p a d", a=4), zps.rearrange("p (a d) -> p a d", a=4),
                     sa[:, 4:8].unsqueeze(2).to_broadcast([128, 4, 64]), ALU.mult, [zbuf, sbf], [q32b])
                P.tt("pool", q32.rearrange("p (a d) -> p a d", a=4), q32.rearrange("p (a d) -> p a d", a=4),
                     qkg[:, gi, :].unsqueeze(1).to_broadcast([128, 4, 64]), ALU.mult, [q32b] + CONST, [q32b])
                qa, qab = qkA.next()
                qb_, qbb = qkB.next()
                v4 = q32.rearrange("p (a h f) -> p a h f", a=4, h=2)
                cosb = ca[:, 0, j, :].unsqueeze(1).to_broadcast([128, 4, 32])
                sinb = ca[:, 1, j, :].unsqueeze(1).to_broadcast([128, 4, 32])
                a4 = qa.rearrange("p (a h f) -> p a h f", a=4, h=2)
                P.tt("dve", a4[:, :, 0, :], v4[:, :, 0, :], cosb, ALU.mult, [q32b, cb], [qab])
                P.tt("pool", a4[:, :, 1, :], v4[:, :, 1, :], cosb, ALU.mult, [q32b, cb], [qab])
                b3 = qb_.rearrange("p (a f) -> p a f", a=4)
                o16, o16b = qk16.next()
                o4 = o16.rearrange("p h (m t f) -> p (h m) t f", m=2, t=2)
                P.tt("pool", b3, v4[:, :, 1, :], sinb, ALU.mult, [q32b, cb], [qbb])
                P.tt("dve", o4[:, :, 0, :], a4[:, :, 0, :], b3, ALU.subtract, [qab, qbb], [o16b])
                P.tt("pool", b3, v4[:, :, 0, :], sinb, ALU.mult, [q32b, cb, o16b], [qbb])
                P.tt("dve", o4[:, :, 1, :], a4[:, :, 1, :], b3, ALU.add, [qab, qbb], [o16b])
                return o16, o16b

            def qk_norm_rope_gen(zps, zbuf, gi, ca, cb, j, res):
                sa, sbf = st_q.next()
                q32, q32b = qk32.next()
                q3 = q32.rearrange("p (a d) -> p a d", a=4)
                P.act(q32, zps, AF.Square, [zbuf], [q32b])
                yield
                P.op("dve", lambda e: e.tensor_reduce(out=sa[:, 0:4], in_=q3, axis=AX.X, op=ALU.add), [q32b], [sbf])
                P.rstd(sa[:, 4:8], sa[:, 0:4], 64, [sbf], [sbf])
                yield
                P.tt("dve", q3, zps.rearrange("p (a d) -> p a d", a=4),
                     sa[:, 4:8].unsqueeze(2).to_broadcast([128, 4, 64]), ALU.mult, [zbuf, sbf], [q32b])
                yield
                P.tt("pool", q3, q3, qkg[:, gi, :].unsqueeze(1).to_broadcast([128, 4, 64]), ALU.mult, [q32b] + CONST, [q32b])
                yield
                qa, qab = qkA.next()
                qb_, qbb = qkB.next()
                v4 = q32.rearrange("p (a h f) -> p a h f", a=4, h=2)
                cosb = ca[:, 0, j, :].unsqueeze(1).to_broadcast([128, 4, 32])
                sinb = ca[:, 1, j, :].unsqueeze(1).to_broadcast([128, 4, 32])
                a4 = qa.rearrange("p (a h f) -> p a h f", a=4, h=2)
                P.tt("dve", a4[:, :, 0, :], v4[:, :, 0, :], cosb, ALU.mult, [q32b, cb], [qab])
                P.tt("pool", a4[:, :, 1, :], v4[:, :, 1, :], cosb, ALU.mult, [q32b, cb], [qab])
                b3 = qb_.rearrange("p (a f) -> p a f", a=4)
                o16, o16b = qk16.next()
                o4 = o16.rearrange("p h (m t f) -> p (h m) t f", m=2, t=2)
                P.tt("pool", b3, v4[:, :, 1, :], sinb, ALU.mult, [q32b, cb], [qbb])
                yield
                P.tt("dve", o4[:, :, 0, :], a4[:, :, 0, :], b3, ALU.subtract, [qab, qbb], [o16b])
                P.tt("pool", b3, v4[:, :, 0, :], sinb, ALU.mult, [q32b, cb, o16b], [qbb])
                yield
                P.tt("dve", o4[:, :, 1, :], a4[:, :, 1, :], b3, ALU.add, [qab, qbb], [o16b])
                res.append((o16, o16b))

            def chain(*gens):
                for g_ in gens:
                    yield from g_

            def lockstep(gens):
                gens = list(gens)
                while gens:
                    nxt = []
                    for gz in gens:
                        try:
                            next(gz)
                            nxt.append(gz)
                        except StopIteration:
                            pass
                    gens = nxt
                    yield

            for sw in range(2):
                h0 = 2 * sw
                kcol = 1024 + h0 * 128
                vcol = 1536 + h0 * 128
                qcol = 512 + h0 * 128

                def qkv_tile(kind, src_d, tile_idx, j, xa_, xb_, ca, cb, dst, dstbuf, vblk=None):
                    ld = nt_load(src_d, tile_idx * 128)
                    yield
                    yield from nt_compute(ld, xa_, xb_, j)
                    zb = ZB[j % 2]
                    col = kcol if kind == "k" else qcol
                    for K in range(8):
                        P.mm(psum_t[zb][:, 0:256], xa_[:, K, j * 128:(j + 1) * 128], w_in16[:, K, col:col + 256],
                             K == 0, K == 7, [xb_, Bwin], [PB[zb]])
                    if kind == "k":
                        for K in range(8):
                            P.mm(psum_t[zb][:, 256:512], xa_[:, K, j * 128:(j + 1) * 128], w_in16[:, K, vcol:vcol + 256],
                                 K == 0, K == 7, [xb_, Bwin], [PB[zb]])
                    yield
                    if kind == "k":
                        P.cp("act", Vp[:, tile_idx, :, 0:128], psum_t[zb][:, 256:512].rearrange("p (h e) -> p h e", h=2),
                             [PB[zb]], [BV[vblk]])
                    res = []
                    yield from qk_norm_rope_gen(psum_t[zb][:, 0:256], PB[zb], 1 if kind == "k" else 0, ca, cb, j, res)
                    o16, o16b = res[0]
                    yield
                    for hh in range(2):
                        P.mm(psum_t[TPB][:, hh * 128:(hh + 1) * 128], o16[:, hh, :], ident, True, True,
                             [o16b] + CONST, [PB[TPB]])
                    c0 = tile_idx * 128 if kind == "k" else j * 128
                    P.cp("act", dst[:, :, c0:c0 + 128],
                         psum_t[TPB][:, 0:256].rearrange("p (h e) -> p h e", h=2), [PB[TPB]], [dstbuf])
                    yield

                def kv_block(blk):
                    xa_, xb_ = xnT.next()
                    ca, cb = cos_sin(posf_s, blk * 4)
                    yield
                    for pair in ((0, 1), (2, 3)):
                        yield from lockstep([qkv_tile("k", xs_d, blk * 4 + j, j, xa_, xb_, ca, cb, KT, BKT[blk], vblk=blk)
                                             for j in pair])

                def pool_gen(s, xa_, xb_):
                    xh_t, Bxh = h1t.next()
                    P.memset("pool", xh_t, 0.0, [Bxh])
                    P.dma(xh_t[112:128, :], xh_d[s * 16:(s + 1) * 16, :], W=[Bxh], sbuf=Bxh)
                    P.dma(bfirst, bfirst_d[s], W=[Bbf], sbuf=Bbf)
                    ha_, hb_ = haloT, BhaloT
                    yield
                    sa, sbf = st_n.next()
                    P.act(junk, xh_t, AF.Square, [Bxh], [sbf], accum=sa[:, 0:1])
                    yield
                    P.rstd(sa[:, 1:2], sa[:, 0:1], 1024, [sbf], [sbf])
                    na, nb = xn16.next()
                    P.ts("dve", na, xh_t, sa[:, 1:2], ALU.mult, [Bxh, sbf], [nb])
                    yield
                    for half in range(2):
                        for kk in range(4):
                            K = half * 4 + kk
                            P.mm(psum_t[TPB][:, kk * 128:(kk + 1) * 128], na[:, K * 128:(K + 1) * 128], ident,
                                 True, True, [nb] + CONST, [PB[TPB]])
                        P.cp("act" if half == 0 else "dve", ha_[:, half * 4:(half + 1) * 4, 0:128],
                             psum_t[TPB][:, :].rearrange("p (k e) -> p k e", k=4), [PB[TPB]], [hb_])
                    yield
                    for jj in range(5):
                        zb = ZB[jj % 2]
                        src, srcb, col = (ha_, hb_, 0) if jj == 0 else (xa_, xb_, (jj - 1) * 128)
                        for K in range(8):
                            P.mm(psum_t[zb][:, :], src[:, K, col:col + 128], w_in16[:, K, 0:512], K == 0, K == 7,
                                 [srcb, Bwin], [PB[zb]])
                        yield
                        P.cp("act" if jj % 2 else "dve", zt[:, jj, :], psum_t[zb][:, :], [PB[zb]], [Bzt])
                        yield
                    for g in range(4):
                        zb = ZB[g % 2]
                        for j in range(4):
                            cur = bfirst[:, g, :] if j == 0 else bgen[:, g, 1, :]
                            P.mm(psum_t[zb][:, j * 128:(j + 1) * 128], zt[:, j, g * 128:(g + 1) * 128], bgen[:, g, 0, :],
                                 True, False, [Bzt] + CONST, [PB[zb]])
                            P.mm(psum_t[zb][:, j * 128:(j + 1) * 128], zt[:, j + 1, g * 128:(g + 1) * 128], cur,
                                 False, True, [Bzt, Bbf] + CONST, [PB[zb]])
                        yield
                        ma_, mb_ = mT.next()
                        P.cp("act", ma_, psum_t[zb][:, :], [PB[zb]], [mb_])
                        yield
                        P.mm(psum_t[TPB][:, :], poolw[:, g, :], ma_, True, True, [mb_] + CONST, [PB[TPB]])
                        P.cp("dve", yTb[:, g, :], psum_t[TPB][:, :], [PB[TPB]], [ByTb])
                        yield

                def own_slot(s, side=None):
                    xa_, xb_ = xnT.next()
                    ca, cb = cos_sin(posf_o, s * 4)
                    qT, qTb = QT.next()
                    ma, mb = msk_r.next()
                    P.dma(ma, amask_d[s].rearrange("k p q -> p k q"), W=[mb], sbuf=mb)
                    for pair in ((0, 1), (2, 3)):
                        for _ in lockstep([qkv_tile("q", xo_d, s * 4 + j, j, xa_, xb_, ca, cb, qT, qTb) for j in pair]):
                            pass
                    if sw == 1:
                        side = chain(pool_gen(s, xa_, xb_), side) if side is not None else pool_gen(s, xa_, xb_)
                    nkb = 2 * s + 2
                    n_side = 3 if s < 2 else (2 if s < 4 else 1)
                    for hh in range(2):
                        nkt = nkb * 4

                        def emit_pv(g, lst):
                            kb = g // 4
                            for m, pa, pb_ in lst:
                                for qt in range(4):
                                    ob, oc = O_SLOT[qt * 2 + m]
                                    P.mm(psum_t[ob][:, oc:oc + 130], pa[:, qt * 128:(qt + 1) * 128], Vp[:, g, hh, :],
                                         g == 0 and (qt * 2 + m) in (0, 4, 6), g == nkt - 1, [pb_, BV[kb], Bones], [PB[ob]],
                                         skip=True)

                        pend = None
                        for g in range(nkt):
                            kb = g // 4
                            cur = []
                            for m in range(2):
                                sb_i = SB_[m]
                                P.mm(psum_t[sb_i][:, :], KT[m * 64:(m + 1) * 64, hh, g * 128:(g + 1) * 128],
                                     qT[m * 64:(m + 1) * 64, hh, :], True, True, [BKT[kb], qTb], [PB[sb_i]])
                                pa, pb_ = pt_r.next()
                                P.act(pa, psum_t[sb_i][:, :], AF.Exp, [PB[sb_i]], [pb_], scale=0.125)
                                if kb >= 2 * s:
                                    P.tt("dve" if m == 0 else "pool", pa, pa, ma[:, g - 8 * s, :], ALU.mult, [pb_, mb], [pb_])
                                cur.append((m, pa, pb_))
                            if pend is not None:
                                emit_pv(*pend)
                            pend = (g, cur)
                            if side is not None:
                                for _ in range(n_side):
                                    next(side, None)
                        emit_pv(*pend)
                        def fin_tile(qt, hh=hh):
                            ob1, oc1 = O_SLOT[qt * 2]
                            ob2, oc2 = O_SLOT[qt * 2 + 1]
                            sa, sbf = st_r.next()
                            P.recip(sa[:, 0:1], psum_t[ob1][:, oc1 + 128:oc1 + 129], [PB[ob1]], [sbf])
                            P.recip(sa[:, 1:2], psum_t[ob2][:, oc2 + 128:oc2 + 129], [PB[ob2]], [sbf])
                            yield
                            P.tt("dve", sa[:, 2:3], sa[:, 1:2], nlam, ALU.mult, [sbf] + CONST, [sbf])
                            oa, oab = o_r.next()
                            P.ts("dve", oa, psum_t[ob1][:, oc1:oc1 + 128], sa[:, 0:1], ALU.mult, [PB[ob1], sbf], [oab])
                            yield
                            P.stt("dve", oa, psum_t[ob2][:, oc2:oc2 + 128], sa[:, 2:3], oa, ALU.mult, ALU.add,
                                  [PB[ob2], sbf, oab], [oab])
                            yield
                            P.act(junk[:, 0:128], oa, AF.Square, [oab], [sbf], accum=sa[:, 3:4])
                            yield
                            P.rstd(sa[:, 4:5], sa[:, 3:4], 128, [sbf], [sbf])
                            yield
                            ya, yb = y16_r.next()
                            P.ts("dve", ya, oa, sa[:, 4:5], ALU.mult, [oab, sbf], [yb])
                            yield
                            P.mm(psum_t[TPB][:, 256:384], ya, ident, True, True, [yb] + CONST, [PB[TPB]])
                            if sw == 0:
                                P.cp("act", yT01[:, hh, s * 512 + qt * 128:s * 512 + (qt + 1) * 128],
                                     psum_t[TPB][:, 256:384], [PB[TPB]], [ByT01[s]])
                            else:
                                P.cp("act", yTb[:, 4 + hh, qt * 128:(qt + 1) * 128], psum_t[TPB][:, 256:384],
                                     [PB[TPB]], [ByTb])
                            yield

                        for pair in ((0, 1), (2, 3)):
                            for _ in lockstep([fin_tile(qt) for qt in pair]):
                                pass
                    if side is not None:
                        for _ in side:
                            pass
                    if sw == 1:
                        for qt in range(4):
                            ha, hb = h1t.next()
                            P.dma(ha, xo_d[(s * 4 + qt) * 128:(s * 4 + qt + 1) * 128, :], W=[hb], sbuf=hb)
                            for half in range(2):
                                zb = ZB[half]
                                for c in range(8):
                                    if c in (4, 5):
                                        lhs = yT01[:, c - 4, s * 512 + qt * 128:s * 512 + (qt + 1) * 128]
                                        rb = ByT01[s]
                                    else:
                                        lhs = yTb[:, c if c < 4 else c - 2, qt * 128:(qt + 1) * 128]
                                        rb = ByTb
                                    P.mm(psum_t[zb][:, :], lhs, w_o16[:, c, half * 512:(half + 1) * 512], c == 0, c == 7,
                                         [rb, Bwo], [PB[zb]])
                            for half in range(2):
                                P.tt("dve", ha[:, half * 512:(half + 1) * 512], psum_t[ZB[half]][:, :],
                                     ha[:, half * 512:(half + 1) * 512], ALU.add, [PB[ZB[half]], hb], [hb])
                            row = (s * 4 + qt) * 128
                            P.dma(h1_d[row:row + 128, :], ha, R=[hb], W=[Bh1d], sbuf=hb)

                for _ in chain(kv_block(0), kv_block(1)):
                    pass
                for s in range(NS):
                    side = chain(kv_block(2 * s + 2), kv_block(2 * s + 3)) if s + 1 < NS else None
                    own_slot(s, side)
                if sw == 0:
                    dump("KT", KT, BKT)
                    dump("Vp", Vp, BV + [Bones])
                    dump("yT01", yT01, ByT01)
            P.barrier(allbufs)

        final_bufs = []
        if 2 in phases:
            A.reset(persist_off)
            acc = A.alloc([4, 1024], F32)
            Bacc = [newbuf("acc%d" % j) for j in range(4)]
            xnT2 = A.alloc([8, 512], BF16)
            BxnT2 = newbuf("xnT2")
            s2 = A.alloc([4, 8, 128], F32)
            y1 = A.alloc([4, 8, 128], F32)
            Bs2 = [newbuf("s2_%d" % j) for j in range(4)]
            By1 = [newbuf("y1_%d" % j) for j in range(4)]
            kap = A.alloc([4, 8], F32)
            Bkap = [newbuf("kap%d" % j) for j in range(4)]
            skT = A.alloc([16, 128], BF16)
            BskT = newbuf("skT")
            sk32 = ring(2, [128], F32, "sk32")
            sk16 = ring(2, [128], BF16, "sk16")
            off0 = A.off
            ut_r = ring(2, [8, 1024], BF16, "ut")
            off1 = A.off
            v_r = ring(2, [8, 1024], BF16, "vg")
            Wq = arena_t[:, off0:off1].bitcast(BF16).rearrange("p (k c) -> p k c", k=8)
            Wg = arena_t[:, off1:A.off].bitcast(BF16).rearrange("p (k c) -> p k c", k=8)
            BWq = ut_r.bufs()
            BWg = v_r.bufs()
            ga_r = ring(2, [8, 512], BF16, "ga")
            hid_r = ring(1, [8, 512], BF16, "hid")
            Bhid = [newbuf("hid_t%d" % j) for j in range(4)]
            D_r = ring(4, [8, 128], BF16, "Dw")
            E_r = ring(4, [8, 128], BF16, "Ew")
            qTc = ring(2, [512], BF16, "qTc")
            junk2 = A.alloc([1024], BF16)
            Bjunk2 = newbuf("junk2")
            xn16b = ring(2, [1024], BF16, "xn16b")
            st2 = ring(4, [8], F32, "st2")
            tk_v = A.alloc([16, 16], F32)
            tk_tmp = ring(2, [128], F32, "tktmp")
            Btkv = newbuf("tkv")
            cand = A.alloc([8, 256], F32)
            Bcand = newbuf("cand")
            ctmp = ring(2, [256], F32, "ctmp")
            csort = A.alloc([8, 24], F32)
            Bcs = newbuf("csort")
            cexp = A.alloc([8, 16], F32)
            tks = A.alloc([8, 8], F32)
            Btks = newbuf("tks")
            gate_r = ring(1, [1024], F32, "gate")
            p32 = ring(1, [256], F32, "p32")
            p16 = ring(1, [256], BF16, "p16")
            pT = A.alloc([2, 512], BF16)
            BpT = newbuf("pT")
            ot_r = ring(1, [1024], F32, "ot")

            AB = (0, 1)
            GB = ((2, 3), (4, 5))
            OBK = (6, 7)
            TP2 = 2

            for q in range(16):
                sa, sb_ = sk32.next()
                P.dma(sa, sk_d[q], W=[sb_], sbuf=sb_)
                ka, kb_ = sk16.next()
                P.cp("pool", ka, sa, [sb_], [kb_])
                P.mm(psum_t[TP2][:, (q % 4) * 128:(q % 4 + 1) * 128], ka, ident, True, True, [kb_] + CONST, [PB[TP2]])
                if q % 4 == 3:
                    P.cp("act", skT[:, q - 3:q + 1, :], psum_t[TP2][:, :].rearrange("p (k e) -> p k e", k=4),
                         [PB[TP2]], [BskT])

            def norm_T(src_ap, src_bufs, dstT, dstT_buf, j, eng_st=st2):
                sa, sbf = eng_st.next()
                P.act(junk2, src_ap, AF.Square, src_bufs, [sbf], accum=sa[:, 0:1])
                P.rstd(sa[:, 1:2], sa[:, 0:1], 1024, [sbf], [sbf])
                na, nb = xn16b.next()
                P.ts("dve", na, src_ap, sa[:, 1:2], ALU.mult, src_bufs + [sbf], [nb])
                for half in range(2):
                    tb = AB[half]
                    for kk in range(4):
                        K = half * 4 + kk
                        P.mm(psum_t[tb][:, kk * 128:(kk + 1) * 128], na[:, K * 128:(K + 1) * 128], ident, True, True,
                             [nb] + CONST, [PB[tb]])
                    P.cp("act" if half == 0 else "dve", dstT[:, half * 4:(half + 1) * 4, j * 128:(j + 1) * 128],
                         psum_t[tb][:, :].rearrange("p (k e) -> p k e", k=4), [PB[tb]], [dstT_buf])

            for pb in range(NS):
                for j in range(4):
                    row = (pb * 4 + j) * 128
                    P.dma(acc[:, j, :], h1_d[row:row + 128, :], R=[Bh1d], W=[Bacc[j]], sbuf=Bacc[j])
                if pb == 0:
                    P.dma(Wq, wq16_d.rearrange("(k p) e -> p k e", p=128), R=[Bwq16d], W=BWq, sbuf=BWq[0])
                for j in range(4):
                    norm_T(acc[:, j, :], [Bacc[j]], xnT2, BxnT2, j)
                for q in range(16):
                    h, half = q // 2, q % 2
                    ab = AB[q % 2]
                    for K in range(8):
                        P.mm(psum_t[ab][:, :], Wq[:, K, q * 128:(q + 1) * 128], xnT2[:, K, :], K == 0, K == 7,
                             BWq + [BxnT2], [PB[ab]])
                    qa, qb = qTc.next()
                    P.cp("act", qa, psum_t[ab][:, :], [PB[ab]], [qb])
                    gb = GB[q % 2][0]
                    for j in range(4):
                        P.mm(psum_t[gb][:, j * 128:(j + 1) * 128], qa[:, j * 128:(j + 1) * 128], skT[:, q, :], True, True,
                             [qb, BskT], [PB[gb]])
                    dst = (y1 if half == 0 else s2)[:, :, h, :]
                    dbufs = By1 if half == 0 else Bs2
                    P.cp("dve", dst, psum_t[gb][:, :].rearrange("p (j n) -> p j n", j=4), [PB[gb]], dbufs)
                for j in range(4):
                    for q in range(16):
                        h, half = q // 2, q % 2
                        src = (y1 if half == 0 else s2)[:, j, h, :]
                        sbuf_l = [By1[j] if half == 0 else Bs2[j]]
                        ta, tb = tk_tmp.next()
                        P.op("dve", lambda e, o=tk_v[:, q, 0:8], i=src: e.max(out=o, in_=i), sbuf_l, [Btkv])
                        P.op("dve", lambda e, o=ta, r=tk_v[:, q, 0:8], i=src: e.match_replace(
                            out=o, in_to_replace=r, in_values=i, imm_value=-1e30), sbuf_l + [Btkv], [tb])
                        P.op("dve", lambda e, o=tk_v[:, q, 8:16], i=ta: e.max(out=o, in_=i), [tb], [Btkv])
                    tv = tk_v.rearrange("p (h two) k -> p h two k", two=2)
                    P.tt("dve", cand.rearrange("p h (a b) -> p h a b", a=16),
                         tv[:, :, 0, :].unsqueeze(3).to_broadcast([128, 8, 16, 16]),
                         tv[:, :, 1, :].unsqueeze(2).to_broadcast([128, 8, 16, 16]), ALU.add, [Btkv], [Bcand])
                    for h in range(8):
                        c0 = cand[:, h, :]
                        t1, t1b = ctmp.next()
                        t2, t2b = ctmp.next()
                        P.op("dve", lambda e, o=csort[:, h, 0:8], i=c0: e.max(out=o, in_=i), [Bcand], [Bcs])
                        P.op("dve", lambda e, o=t1, r=csort[:, h, 0:8], i=c0: e.match_replace(
                            out=o, in_to_replace=r, in_values=i, imm_value=-1e30), [Bcand, Bcs], [t1b])
                        P.op("dve", lambda e, o=csort[:, h, 8:16], i=t1: e.max(out=o, in_=i), [t1b], [Bcs])
                        P.op("dve", lambda e, o=t2, r=csort[:, h, 8:16], i=t1: e.match_replace(
                            out=o, in_to_replace=r, in_values=i, imm_value=-1e30), [t1b, Bcs], [t2b])
                        P.op("dve", lambda e, o=csort[:, h, 16:24], i=t2: e.max(out=o, in_=i), [t2b], [Bcs])
                    P.tt("dve", tks[:, :, 0], csort[:, :, 15], csort[:, :, 16], ALU.add, [Bcs], [Btks])
                    P.ts("dve", tks[:, :, 0], tks[:, :, 0], 0.5, ALU.mult, [Btks], [Btks])
                    P.tt("dve", cexp, csort[:, :, 0:16], csort[:, :, 0:1].to_broadcast([128, 8, 16]), ALU.subtract,
                         [Bcs], [Btks])
                    P.act(cexp, cexp, AF.Exp, [Btks], [Btks])
                    P.op("dve", lambda e, o=tks[:, :, 1], i=cexp: e.tensor_reduce(out=o, in_=i, axis=AX.X, op=ALU.add),
                         [Btks], [Btks])
                    P.act(tks[:, :, 2], tks[:, :, 1], AF.Ln, [Btks], [Btks])
                    P.tt("dve", tks[:, :, 3], tks[:, :, 0], csort[:, :, 0], ALU.subtract, [Btks, Bcs], [Btks])
                    P.tt("dve", kap[:, j, :], tks[:, :, 3], tks[:, :, 2], ALU.subtract, [Btks], [Bkap[j]])
                    P.tt("dve", y1[:, j, :, :], y1[:, j, :, :], tks[:, :, 0:1].to_broadcast([128, 8, 128]), ALU.subtract,
                         [By1[j], Btks], [By1[j]])
                hda = hid_r.items[0][0]
                gainfo = {}

                uinfo = {}
                vinfo = {}

                def emit_u_dma(g):
                    ua, ub = ut_r.next()
                    P.dma(ua, ut_d[:, g * 1024:(g + 1) * 1024].rearrange("(k p) e -> p k e", p=128), R=[Butd], W=[ub], sbuf=ub)
                    uinfo[g] = (ua, ub)

                def emit_v_dma(g):
                    va, vb = v_r.next()
                    P.dma(va, v16_d[g * 1024:(g + 1) * 1024, :].rearrange("(c p) d -> p c d", p=128), R=[Bv16d], W=[vb], sbuf=vb)
                    vinfo[g] = (va, vb)

                def emit_a_start(g):
                    if g not in uinfo:
                        emit_u_dma(g)
                    if g not in vinfo:
                        emit_v_dma(g)
                    gaa, gab = ga_r.next()
                    gainfo[g] = (gaa, gab) + vinfo[g]

                def emit_a_mm(g, c):
                    ua, ub = uinfo[g]
                    ab = AB[c % 2]
                    for K in range(8):
                        P.mm(psum_t[ab][:, :], ua[:, K, c * 128:(c + 1) * 128], xnT2[:, K, :], K == 0, K == 7,
                             [ub, BxnT2], [PB[ab]])

                def emit_a_cp(g, c):
                    gaa, gab = gainfo[g][0:2]
                    ab = AB[c % 2]
                    P.cp("act", gaa[:, c, :], psum_t[ab][:, :], [PB[ab]], [gab])

                def emit_a_gelu(g):
                    gaa, gab = gainfo[g][0:2]
                    P.act(gaa, gaa, AF.Gelu, [gab], [gab])

                def emit_head(g, j, h):
                    gb0, gb1 = GB[j % 2]
                    da, db = D_r.next()
                    ea, eb = E_r.next()
                    P.tt("dve", da, y1[:, j, h, g * 8:(g + 1) * 8].unsqueeze(2).to_broadcast([128, 8, 128]),
                         s2[:, j, h, :].unsqueeze(1).to_broadcast([128, 8, 128]), ALU.add, [By1[j], Bs2[j]], [db])
                    if h % 2 == 0 or (h == 7 and j % 2 == 1):
                        P.stt("dve", da, da, BIG, da, ALU.mult, ALU.min, [db], [db])
                    else:
                        P.op("act", lambda e, o=da: e.activation(out=o, in_=o, func=AF.Prelu, alpha=BIG), [db], [db])
                    P.act(ea, da, AF.Exp, [db, Bkap[j]], [eb], bias=kap[:, j, h:h + 1])
                    for c in range(GRP):
                        gbk = gb0 if c < 4 else gb1
                        P.mm(psum_t[gbk][:, (c % 4) * 128:(c % 4 + 1) * 128], ea[:, c, :], ident,
                             h == 0 and c % 4 == 0, h == 7, [eb] + CONST, [PB[gbk]], skip=True)

                def emit_hid(g, j):
                    gaa, gab, va, vb = gainfo[g]
                    for half in range(2):
                        gbk = GB[j % 2][half]
                        P.tt("dve", hda[:, half * 4:(half + 1) * 4, j * 128:(j + 1) * 128],
                             gaa[:, half * 4:(half + 1) * 4, j * 128:(j + 1) * 128],
                             psum_t[gbk][:, :].rearrange("p (c t) -> p c t", c=4), ALU.mult, [gab, PB[gbk]], [Bhid[j]])

                def emit_O(g, j):
                    gaa, gab, va, vb = gainfo[g]
                    for half in range(2):
                        ob = OBK[half]
                        for c in range(GRP):
                            P.mm(psum_t[ob][:, :], hda[:, c, j * 128:(j + 1) * 128], va[:, c, half * 512:(half + 1) * 512],
                                 c == 0, c == GRP - 1, [Bhid[j], vb], [PB[ob]])

                def emit_acc(g, j):
                    for half in range(2):
                        ob = OBK[half]
                        P.tt("dve", acc[:, j, half * 512:(half + 1) * 512], acc[:, j, half * 512:(half + 1) * 512],
                             psum_t[ob][:, :], ALU.add, [Bacc[j], PB[ob]], [Bacc[j]])

                emit_a_start(0)
                for c in range(GRP):
                    emit_a_mm(0, c)
                    emit_a_cp(0, c)
                emit_a_gelu(0)
                steps = [(g, j) for g in range(NGRP) for j in range(4)]
                prev = None
                for (g, j) in steps:
                    for h in range(8):
                        emit_head(g, j, h)
                        if prev is not None and h == 1:
                            emit_hid(*prev)
                            emit_O(*prev)
                        if prev is not None and h == 4:
                            emit_acc(*prev)
                        if j == 0 and h == 0 and g + 1 < NGRP:
                            emit_u_dma(g + 1)
                        if j == 0 and h == 3 and g + 1 < NGRP:
                            emit_v_dma(g + 1)
                        if g + 1 < NGRP:
                            if j == 1 and h == 0:
                                emit_a_start(g + 1)
                            if j in (1, 2):
                                c_ = (j - 1) * 4 + h // 2
                                if h % 2 == 0:
                                    emit_a_mm(g + 1, c_)
                                else:
                                    emit_a_cp(g + 1, c_)
                            if j == 3 and h == 3:
                                emit_a_gelu(g + 1)
                        if g == NGRP - 1 and j == 1 and h == 0 and pb + 1 < NS:
                            P.dma(Wq, wq16_d.rearrange("(k p) e -> p k e", p=128), R=[Bwq16d], W=BWq, sbuf=BWq[0])
                    prev = (g, j)
                emit_hid(*prev)
                emit_O(*prev)
                emit_acc(*prev)
                P.dma(Wg[:, :, 0:1024], wg16_d.rearrange("(k p) e -> p k e", p=128), R=[Bwg16d], W=BWg, sbuf=BWg[0])
                P.dma(Wg[:, 0:2, 1024:2048], wp16_d.rearrange("(k p) e -> p k e", p=128), R=[Bwp16d], W=BWg, sbuf=BWg[0])
                for j in range(4):
                    norm_T(acc[:, j, :], [Bacc[j]], xnT2, BxnT2, j)
                for j in range(4):
                    row = (pb * 4 + j) * 128
                    pa, pb_ = p32.next()
                    P.dma(pa, po_d[row:row + 128, :], W=[pb_], sbuf=pb_)
                    p6, p6b = p16.next()
                    P.cp("pool", p6, pa, [pb_], [p6b])
                    for K in range(2):
                        P.mm(psum_t[GB[0][0]][:, K * 128:(K + 1) * 128], p6[:, K * 128:(K + 1) * 128], ident, True, True,
                             [p6b] + CONST, [PB[GB[0][0]]])
                    P.cp("act", pT[:, :, j * 128:(j + 1) * 128],
                         psum_t[GB[0][0]][:, 0:256].rearrange("p (k e) -> p k e", k=2), [PB[GB[0][0]]], [BpT])
                for j in range(4):
                    ga_, gb_ = gate_r.next()
                    oa, ob_ = ot_r.next()
                    for half in range(2):
                        ab = AB[half]
                        for K in range(8):
                            P.mm(psum_t[ab][:, :], xnT2[:, K, j * 128:(j + 1) * 128], Wg[:, K, half * 512:(half + 1) * 512],
                                 K == 0, K == 7, [BxnT2] + BWg, [PB[ab]])
                        P.act(ga_[:, half * 512:(half + 1) * 512], psum_t[ab][:, :], AF.Sigmoid, [PB[ab]], [gb_])
                        ob = OBK[half]
                        for K in range(2):
                            P.mm(psum_t[ob][:, :], pT[:, K, j * 128:(j + 1) * 128],
                                 Wg[:, K, 1024 + half * 512:1024 + (half + 1) * 512], K == 0, K == 1, [BpT] + BWg, [PB[ob]])
                        P.tt("dve", ga_[:, half * 512:(half + 1) * 512], ga_[:, half * 512:(half + 1) * 512], psum_t[ob][:, :],
                             ALU.mult, [gb_, PB[ob]], [gb_])
                        P.tt("pool", oa[:, half * 512:(half + 1) * 512], ga_[:, half * 512:(half + 1) * 512],
                             acc[:, j, half * 512:(half + 1) * 512], ALU.add, [gb_, Bacc[j]], [ob_])
                    row = (pb * 4 + j) * 128
                    P.dma(out_d[row:row + 128, :], oa, R=[ob_], sbuf=ob_)
                final_bufs = ot_r.bufs()
        if debug and 2 not in phases:
            A.reset(persist_off)
            da_, db_ = A.alloc([1024], F32), newbuf("dbg")
            for r0 in range(0, NO, 128):
                P.dma(da_, h1_d[r0:r0 + 128, :], R=[Bh1d], W=[db_], sbuf=db_)
                P.dma(dbg_d[r0:r0 + 128, :], da_, R=[db_], sbuf=db_)
            final_bufs = [db_]
        A.reset(A.off)
        print('arena high-water words', A.hw, 'of', AW)
        P.emit(final_bufs=list(final_bufs) + dumps)
    return nc


POOL_WINDOWS = (2, 4, 8, 16)


def own_blocks(r, nblk):
    out = []
    for s in range(nblk // 2):
        lo = (s % 2 == 0)
        if r == 0:
            out.append(2 * s if lo else 2 * s + 1)
        else:
            out.append(2 * s + 1 if lo else 2 * s)
    return out


def host_consts(S):
    bf = ml_dtypes.bfloat16
    NS = S // 1024
    t = np.arange(128)
    bgen = np.zeros((128, 4, 2, 128), np.float32)
    for g, w in enumerate(POOL_WINDOWS):
        cur = ((t[:, None] <= t[None, :]) & (t[:, None] > t[None, :] - w)).astype(np.float32) / w - np.eye(128, dtype=np.float32)
        prev = ((t[:, None] - 128 > t[None, :] - w)).astype(np.float32) / w
        bgen[:, g, 0, :] = prev
        bgen[:, g, 1, :] = cur
    return bgen.astype(bf)


def core_inputs(b, r, S, x, p, positions, shared):
    bf = ml_dtypes.bfloat16
    NS = S // 1024
    nblk = S // 512
    blocks = own_blocks(r, nblk)
    xs = np.ascontiguousarray(x[b])
    pos = positions[b].astype(np.int32)
    own_idx = np.concatenate([np.arange(k * 512, (k + 1) * 512) for k in blocks])
    xo = np.ascontiguousarray(xs[own_idx])
    po = np.ascontiguousarray(p[0, b][own_idx])
    xh = np.zeros((NS * 16, D), np.float32)
    t = np.arange(128)
    bfirst = np.zeros((NS, 128, 4, 128), np.float32)
    amask = np.zeros((NS, 8, 128, 512), np.float32)
    for s, k in enumerate(blocks):
        if k > 0:
            xh[s * 16:(s + 1) * 16] = xs[k * 512 - 16:k * 512]
        for g, w in enumerate(POOL_WINDOWS):
            band = ((t[:, None] <= t[None, :]) & (t[:, None] > t[None, :] - w)).astype(np.float32)
            if k == 0:
                cnt = np.minimum(t + 1, w).astype(np.float32)
                bfirst[s, :, g, :] = band / cnt[None, :] - np.eye(128, dtype=np.float32)
            else:
                bfirst[s, :, g, :] = band / w - np.eye(128, dtype=np.float32)
        qpos = k * 512 + np.arange(512)
        for kk in range(8):
            kpos = (2 * s) * 512 + kk * 128 + np.arange(128)
            amask[s, kk] = (kpos[:, None] <= qpos[None, :]).astype(np.float32)
    d = dict(shared)
    d.update(
        xs=xs, pos_s=np.ascontiguousarray(pos.reshape(S // 128, 128).T),
        xo=xo, pos_o=np.ascontiguousarray(pos[own_idx].reshape(NS * 4, 128).T),
        xh=xh, amask=amask.astype(bf), bfirst=bfirst.astype(bf), po=po,
    )
    return d, own_idx


def shared_inputs(S, ln_mix, w_in, pool_w, pool_scale, q_norm, k_norm, lambda_q1, lambda_k1, lambda_q2, lambda_k2,
                  subln, w_o, ln_ffn, w_peer_q, peer_subkeys, peer_u, peer_v, ln_pe, w_pe_gate, w_pe_proj):
    f = lambda a: np.ascontiguousarray(np.asarray(a, np.float32))
    colT = lambda v: f(np.asarray(v, np.float32).reshape(-1, 128).T)
    rep = lambda v: f(np.broadcast_to(np.asarray(v, np.float32), (128,) + np.asarray(v).shape))
    rowsc = np.concatenate([colT(pool_scale[0]), np.asarray(subln[0], np.float32).reshape(128, 1).repeat(4, axis=1)], axis=1)
    inv_freq = (10000.0 ** (-np.arange(0, 64, 2, dtype=np.float32) / 64)).astype(np.float32)
    return dict(
        bgen=host_consts(S),
        ln_mix_t=colT(ln_mix[0]), w_in=f(w_in[0]), pool_w=f(pool_w[0]), rowscale_t=f(rowsc),
        qk_gain=f(np.stack([rep(q_norm[0]), rep(k_norm[0])], axis=1)),
        lam_vecs=f(np.stack([rep(lambda_q1[0]), rep(lambda_k1[0]), rep(lambda_q2[0]), rep(lambda_k2[0])], axis=1)),
        w_o=f(w_o[0]), ln_ffn_t=colT(ln_ffn[0]), w_peer_q=f(w_peer_q[0]),
        peer_subkeys=f(np.asarray(peer_subkeys[0], np.float32).reshape(16, 128, 128)),
        peer_u=f(peer_u[0]), peer_v=f(peer_v[0]), ln_pe_t=colT(ln_pe[0]), w_pe_gate=f(w_pe_gate[0]),
        w_pe_proj=f(w_pe_proj[0]), ident=np.eye(128, dtype=np.float32), invfreq=rep(inv_freq),
    )


def run(x, p, positions, weights, phases=(0, 1, 2), debug=False):
    x = np.asarray(x, np.float32)
    p = np.asarray(p, np.float32)
    positions = np.asarray(positions)
    B, S, _ = x.shape
    shared = shared_inputs(S, **weights)
    nc = build(S, phases=phases, debug=debug)
    in_maps, idxs = [], []
    for b in range(B):
        for r in range(2):
            d, own_idx = core_inputs(b, r, S, x, p, positions, shared)
            in_maps.append(d)
            idxs.append((b, own_idx))
    res = run_bass_kernel_spmd(nc, in_maps, core_ids=list(range(2 * B)))
    out = np.zeros((B, S, D), np.float32)
    key = "dbg" if (debug and 2 not in phases) else "out"
    for (b, own_idx), r in zip(idxs, res.results):
        out[b, own_idx] = r[key]
    if debug:
        return out, res.results
    return out


def kernel(x, p, positions, **weights):
    return run(x, p, positions, weights)
```
